# Optimizing a Trainium2 kernel written in Bass

```python
import math
import jax, jax.numpy as jnp
from jax import lax
import numpy as np

D_MODEL = 1024
BATCH = 8
SEQ = 2048
DEPTH = 4

D_MIX = D_MODEL
N_MIXERS = 4
GROUP_W = D_MIX // N_MIXERS
CONF_KERNEL = 31
CONF_GROUPS = 4
GN_EPS = 1e-5
SCONV_KERNEL = 3
ATT_HEADS = 4
HEAD_DIM = GROUP_W // ATT_HEADS
MOBA_BLOCK = 256
MOBA_TOP_K = 3
Q_BLOCK = 128
LRU_BLOCKS = 4
LRU_BLOCK_W = GROUP_W // LRU_BLOCKS
LRU_CONV = 4
LRU_C = 8.0
D_FF = 4 * D_MODEL
RMS_EPS = 1e-6
CONF_COLS = 2 * GROUP_W
SC_COLS = 3 * GROUP_W
ATT_COLS = 3 * GROUP_W
LRU_COLS = 2 * GROUP_W
IN_COLS = CONF_COLS + SC_COLS + ATT_COLS + LRU_COLS

kernel_name = "hybrid_parallel_groups_moba_rglru_conv"


def rms_norm(x, g):
    xf = x.astype(jnp.float32)
    y = xf * lax.rsqrt(jnp.mean(xf * xf, axis=-1, keepdims=True) + RMS_EPS)
    return (y * g.astype(jnp.float32)).astype(x.dtype)


def causal_depthwise_conv(x, w):
    k_w, c = w.shape
    return lax.conv_general_dilated(
        x, w[:, None, :].astype(x.dtype), window_strides=(1,), padding=[(k_w - 1, 0)],
        dimension_numbers=("NWC", "WIO", "NWC"), feature_group_count=c)


def group_norm_channels(x, g, b):
    bsz, s, c = x.shape
    xf = x.astype(jnp.float32).reshape(bsz, s, CONF_GROUPS, c // CONF_GROUPS)
    mu = jnp.mean(xf, axis=-1, keepdims=True)
    var = jnp.mean(jnp.square(xf - mu), axis=-1, keepdims=True)
    y = ((xf - mu) * lax.rsqrt(var + GN_EPS)).reshape(bsz, s, c)
    return (y * g.astype(jnp.float32) + b.astype(jnp.float32)).astype(x.dtype)


def conformer_conv_mixer(u, dw_w, dw_b, gn_g, gn_b):
    val, gate = jnp.split(u, 2, axis=-1)
    z = val * jax.nn.sigmoid(gate)
    z = causal_depthwise_conv(z, dw_w) + dw_b
    z = group_norm_channels(z, gn_g, gn_b)
    return jax.nn.silu(z)


def short_conv_mixer(u, conv_w):
    b_gate, c_gate, xs = jnp.split(u, 3, axis=-1)
    return b_gate * causal_depthwise_conv(c_gate * xs, conv_w)


def block_diag(x, w, b):
    bsz, s, c = x.shape
    xg = x.reshape(bsz, s, LRU_BLOCKS, LRU_BLOCK_W)
    return jnp.einsum("bsgi,gij->bsgj", xg, w).reshape(bsz, s, c) + b


def rglru_mixer(u, conv_w, conv_b, wa, ba, wx, bx, lam):
    xr, gate = jnp.split(u, 2, axis=-1)
    xc = causal_depthwise_conv(xr, conv_w) + conv_b
    r = jax.nn.sigmoid(block_diag(xc, wa, ba).astype(jnp.float32))
    i = jax.nn.sigmoid(block_diag(xc, wx, bx).astype(jnp.float32))
    log_a = -LRU_C * r * jax.nn.softplus(-lam.astype(jnp.float32))
    a = jnp.exp(log_a)
    mult = jnp.sqrt(-jnp.expm1(2.0 * log_a))
    bterm = mult * (i * xc.astype(jnp.float32))

    def combine(e1, e2):
        a1, b1 = e1
        a2, b2 = e2
        return a1 * a2, a2 * b1 + b2

    _, h = lax.associative_scan(combine, (a, bterm), axis=1)
    return (h * jax.nn.gelu(gate.astype(jnp.float32))).astype(u.dtype)


def moba_attention(u):
    bsz, s, _ = u.shape
    q, k, v = jnp.split(u.astype(jnp.float32), 3, axis=-1)
    q = q.reshape(bsz, s, ATT_HEADS, HEAD_DIM).transpose(0, 2, 1, 3) * (HEAD_DIM ** -0.5)
    k = k.reshape(bsz, s, ATT_HEADS, HEAD_DIM)
    v = v.reshape(bsz, s, ATT_HEADS, HEAD_DIM)
    nb = -(-s // MOBA_BLOCK)
    pad = nb * MOBA_BLOCK - s
    kp = jnp.pad(k, ((0, 0), (0, pad), (0, 0), (0, 0)))
    vp = jnp.pad(v, ((0, 0), (0, pad), (0, 0), (0, 0)))
    kb = kp.reshape(bsz, nb, MOBA_BLOCK, ATT_HEADS, HEAD_DIM).transpose(0, 3, 1, 2, 4)
    vb = vp.reshape(bsz, nb, MOBA_BLOCK, ATT_HEADS, HEAD_DIM).transpose(0, 3, 1, 2, 4)
    slopes = 2.0 ** (-8.0 * jnp.arange(1, ATT_HEADS + 1, dtype=jnp.float32) / ATT_HEADS)

    kmean = jnp.mean(kb, axis=3)
    gate = jnp.einsum("bhsd,bhnd->bhsn", q, kmean)
    q_blk = jnp.arange(s) // MOBA_BLOCK
    past = jnp.arange(nb)[None, :] < q_blk[:, None]
    gate = jnp.where(past, gate, -jnp.inf)
    topk = max(min(MOBA_TOP_K, nb - 1), 1)
    _, idx = lax.top_k(gate, topk)

    nq = s // Q_BLOCK
    q_c = q.reshape(bsz, ATT_HEADS, nq, Q_BLOCK, HEAD_DIM).transpose(0, 2, 1, 3, 4)
    q_c = q_c.reshape(bsz * nq, ATT_HEADS, Q_BLOCK, HEAD_DIM)
    i_c = idx.reshape(bsz, ATT_HEADS, nq, Q_BLOCK, topk).transpose(0, 2, 1, 3, 4)
    i_c = i_c.reshape(bsz * nq, ATT_HEADS, Q_BLOCK, topk)
    b_ids = jnp.repeat(jnp.arange(bsz), nq)
    n_ids = jnp.tile(jnp.arange(nq), bsz)
    hh = jnp.arange(ATT_HEADS)[:, None, None]
    offs = jnp.arange(MOBA_BLOCK)

    def one_block(args):
        qc, ic, b, n = args
        kbb = kb[b]
        vbb = vb[b]
        t = n * Q_BLOCK + jnp.arange(Q_BLOCK)
        own = (n * Q_BLOCK) // MOBA_BLOCK
        k_own = kbb[:, own]
        v_own = vbb[:, own]
        s_pos = own * MOBA_BLOCK + offs
        d_own = (t[:, None] - s_pos[None, :]).astype(jnp.float32)
        s_own = jnp.einsum("hqd,hkd->hqk", qc, k_own) - slopes[:, None, None] * d_own
        s_own = jnp.where(d_own >= 0, s_own, -jnp.inf)
        kg = kbb[hh, ic]
        vg = vbb[hh, ic]
        sel_pos = ic[..., None] * MOBA_BLOCK + offs
        d_sel = (t[None, :, None, None] - sel_pos).astype(jnp.float32)
        s_sel = jnp.einsum("hqd,hqjkd->hqjk", qc, kg) - slopes[:, None, None, None] * d_sel
        s_sel = jnp.where((ic < own)[..., None], s_sel, -jnp.inf)
        scores = jnp.concatenate([s_sel.reshape(ATT_HEADS, Q_BLOCK, topk * MOBA_BLOCK), s_own], axis=-1)
        p = jax.nn.softmax(scores, axis=-1)
        p_sel = p[..., : topk * MOBA_BLOCK].reshape(ATT_HEADS, Q_BLOCK, topk, MOBA_BLOCK)
        p_own = p[..., topk * MOBA_BLOCK:]
        return (jnp.einsum("hqjk,hqjkd->hqd", p_sel, vg)
                + jnp.einsum("hqk,hkd->hqd", p_own, v_own))

    out = lax.map(one_block, (q_c, i_c, b_ids, n_ids))
    out = out.reshape(bsz, nq, ATT_HEADS, Q_BLOCK, HEAD_DIM).transpose(0, 1, 3, 2, 4)
    return out.reshape(bsz, s, GROUP_W).astype(u.dtype)


def setup_inputs(seed: int = 0) -> dict:
    key = jax.random.key(seed)
    ks = jax.random.split(key, 24)
    f32 = jnp.float32

    def nrm(k, shape, scale):
        return jax.random.normal(k, shape, f32) * scale

    def gain(k, shape):
        return 1.0 + 0.05 * jax.random.normal(k, shape, f32)

    u = jax.random.uniform(ks[20], (DEPTH, GROUP_W), f32, 0.9, 0.999)
    base = u ** (1.0 / LRU_C)
    lru_lam = jnp.log(base) - jnp.log1p(-base)
    return {
        "x": jax.random.normal(ks[0], (BATCH, SEQ, D_MODEL), f32),
        "pre_mix_g": gain(ks[1], (DEPTH, D_MODEL)),
        "w_in": nrm(ks[2], (DEPTH, D_MODEL, IN_COLS), D_MODEL ** -0.5),
        "conf_dw_w": nrm(ks[3], (DEPTH, CONF_KERNEL, GROUP_W), CONF_KERNEL ** -0.5),
        "conf_dw_b": nrm(ks[4], (DEPTH, GROUP_W), 0.02),
        "conf_gn_g": gain(ks[5], (DEPTH, GROUP_W)),
        "conf_gn_b": nrm(ks[6], (DEPTH, GROUP_W), 0.02),
        "sconv_w": nrm(ks[7], (DEPTH, SCONV_KERNEL, GROUP_W), SCONV_KERNEL ** -0.5),
        "lru_conv_w": nrm(ks[8], (DEPTH, LRU_CONV, GROUP_W), LRU_CONV ** -0.5),
        "lru_conv_b": nrm(ks[9], (DEPTH, GROUP_W), 0.02),
        "lru_wa": nrm(ks[10], (DEPTH, LRU_BLOCKS, LRU_BLOCK_W, LRU_BLOCK_W), LRU_BLOCK_W ** -0.5),
        "lru_ba": nrm(ks[11], (DEPTH, GROUP_W), 0.02),
        "lru_wx": nrm(ks[12], (DEPTH, LRU_BLOCKS, LRU_BLOCK_W, LRU_BLOCK_W), LRU_BLOCK_W ** -0.5),
        "lru_bx": nrm(ks[13], (DEPTH, GROUP_W), 0.02),
        "lru_lam": lru_lam,
        "w_out": nrm(ks[14], (DEPTH, D_MIX, D_MODEL), D_MIX ** -0.5),
        "post_mix_g": gain(ks[15], (DEPTH, D_MODEL)),
        "pre_mlp_g": gain(ks[16], (DEPTH, D_MODEL)),
        "mlp_w1": nrm(ks[17], (DEPTH, D_MODEL, D_FF), D_MODEL ** -0.5),
        "mlp_w2": nrm(ks[18], (DEPTH, D_FF, D_MODEL), D_FF ** -0.5),
        "post_mlp_g": gain(ks[19], (DEPTH, D_MODEL)),
    }


def reference(x, pre_mix_g, w_in, conf_dw_w, conf_dw_b, conf_gn_g, conf_gn_b, sconv_w,
              lru_conv_w, lru_conv_b, lru_wa, lru_ba, lru_wx, lru_bx, lru_lam, w_out,
              post_mix_g, pre_mlp_g, mlp_w1, mlp_w2, post_mlp_g):
    splits = [CONF_COLS, CONF_COLS + SC_COLS, CONF_COLS + SC_COLS + ATT_COLS]
    for l in range(DEPTH):
        h = rms_norm(x, pre_mix_g[l])
        u = h @ w_in[l]
        u_conf, u_sc, u_att, u_lru = jnp.split(u, splits, axis=-1)
        y_a = conformer_conv_mixer(u_conf, conf_dw_w[l], conf_dw_b[l], conf_gn_g[l], conf_gn_b[l])
        y_b = short_conv_mixer(u_sc, sconv_w[l])
        y_c = moba_attention(u_att)
        y_d = rglru_mixer(u_lru, lru_conv_w[l], lru_conv_b[l], lru_wa[l], lru_ba[l],
                          lru_wx[l], lru_bx[l], lru_lam[l])
        y = jnp.concatenate([y_a, y_b, y_c, y_d], axis=-1) @ w_out[l]
        x = x + rms_norm(y, post_mix_g[l])
        h = rms_norm(x, pre_mlp_g[l])
        m = jnp.square(jax.nn.relu(h @ mlp_w1[l])) @ mlp_w2[l]
        x = x + rms_norm(m, post_mlp_g[l])
    return x
```

```python
import numpy as np
from contextlib import ExitStack
from concourse.bass_utils import run_bass_kernel_spmd
import concourse.bass as bass
import concourse.mybir as mybir

F32 = mybir.dt.float32
BF16 = mybir.dt.bfloat16
AF = mybir.ActivationFunctionType
ALU = mybir.AluOpType
AX = mybir.AxisListType


class Prog:
    ENGS = ("pe", "act", "dve", "pool", "sp")

    def __init__(self, nc):
        self.nc = nc
        self.ops = []
        self.epoch = 0

    def add(self, eng, fn, r=(), w=(), dma=None):
        import sys
        f = sys._getframe(2)
        self.ops.append(dict(eng=eng, fn=fn, r=list(r), w=list(w), dma=dma, epoch=self.epoch, ln=f.f_lineno))

    def pe(self, fn, r=(), w=()):
        self.add("pe", fn, r, w)

    def act(self, fn, r=(), w=()):
        self.add("act", fn, r, w)

    def dve(self, fn, r=(), w=()):
        self.add("dve", fn, r, w)

    def pool(self, fn, r=(), w=()):
        self.add("pool", fn, r, w)

    def dma(self, fn, r=(), w=(), group=None, eng="sp"):
        assert group is not None
        self.add(eng, fn, r, w, dma=group)

    def build(self, stack):
        nc = self.nc
        ops = self.ops
        n = len(ops)
        last_w = {}
        readers = {}
        raw = [set() for _ in range(n)]
        oth = [set() for _ in range(n)]
        for i, op in enumerate(ops):
            rid0 = ("dma", i) if op["dma"] is not None else op["eng"]
            for k in op["r"]:
                if k in last_w:
                    raw[i].add(last_w[k])
                if isinstance(k, tuple) and k[0] == "ps":
                    for rj, j in readers.get(k, {}).items():
                        if rj != rid0:
                            oth[i].add(j)
            for k in op["w"]:
                if k in last_w:
                    oth[i].add(last_w[k])
                for j in readers.get(k, {}).values():
                    oth[i].add(j)
            for k in op["w"]:
                last_w[k] = i
                readers[k] = {}
            rid = ("dma", i) if op["dma"] is not None else op["eng"]
            for k in op["r"]:
                readers.setdefault(k, {})[rid] = i
        need = [[] for _ in range(n)]
        signal = [False] * n
        for i, op in enumerate(ops):
            ds = set()
            for j in raw[i]:
                if j == i:
                    continue
                oj = ops[j]
                if oj["dma"] is None and op["dma"] is None and oj["eng"] == op["eng"] == "pe":
                    continue
                ds.add(j)
            for j in oth[i]:
                if j == i:
                    continue
                oj = ops[j]
                if oj["dma"] is None and op["dma"] is None and oj["eng"] == op["eng"] == "pe":
                    continue
                ds.add(j)
            need[i] = sorted(ds)
            for j in ds:
                signal[j] = True
        for i, op in enumerate(ops):
            if op["dma"] is not None:
                signal[i] = True
        semkeys = {}
        counts = {}
        sigval = [None] * n
        for i, op in enumerate(ops):
            if not signal[i]:
                continue
            key = ("dma", op["dma"]) if op["dma"] is not None else (op["eng"], op["epoch"])
            inc = 16 if op["dma"] is not None else 1
            counts[key] = counts.get(key, 0) + inc
            sigval[i] = (key, counts[key], inc)
        sems = {}
        for key in counts:
            sems[key] = stack.enter_context(nc.semaphore("s_" + "_".join(str(x) for x in key).replace(" ", "")))
        self.nsem = len(sems)
        self.semmap = {str(k): str(v) for k, v in sems.items()}
        import os
        if os.environ.get("DUMPSIG"):
            import json, re
            num = {k: re.search(r"num=(\d+)", str(v)).group(1) for k, v in sems.items()}
            sm = {}
            for i, op in enumerate(ops):
                if sigval[i] is not None:
                    key, val, _ = sigval[i]
                    sm["%s:%d" % (num[key], val)] = [op["ln"], op["eng"], str(key)]
            json.dump(sm, open(os.environ["DUMPSIG"], "w"))
        per_eng = {e: [] for e in self.ENGS}
        for i, op in enumerate(ops):
            per_eng[op["eng"]].append(i)

        def emit_engine(ename, e):
            seen = {}
            for i in per_eng[ename]:
                op = ops[i]
                for j in need[i]:
                    key, val, _ = sigval[j]
                    if seen.get(key, 0) >= val:
                        continue
                    e.wait_ge(sems[key], val)
                    seen[key] = val
                inst = op["fn"](e)
                if signal[i]:
                    key, val, inc = sigval[i]
                    inst.then_inc(sems[key], inc)
            if ename == "sp":
                for key, tot in counts.items():
                    if key[0] == "dma" and seen.get(key, 0) < tot:
                        e.wait_ge(sems[key], tot)

        with nc.Block() as block:
            @block.tensor
            def _(e):
                emit_engine("pe", e)

            @block.scalar
            def _(e):
                emit_engine("act", e)

            @block.vector
            def _(e):
                emit_engine("dve", e)

            @block.gpsimd
            def _(e):
                emit_engine("pool", e)

            @block.sync
            def _(e):
                emit_engine("sp", e)
D = 1024
S = 2048
NL_ALL = 4
NPL = 122
NPAR = NL_ALL * NPL + 64
SPLIT_DMA = False
import os
DBGSKIP = os.environ.get('DBGSKIP', '')
BIG = 32768.0
SLOPES = [2.0 ** (-8.0 * (h + 1) / 4) for h in range(4)]


def build_nc(NL=4, stage=99):
    on = lambda k: 1 if stage >= k else 0
    nc = bass.Bass("TRN2", target_bir_lowering=False)
    dt = nc.dram_tensor
    x_d = dt("x", [S, D], F32, kind="ExternalInput").ap()
    win_d = dt("w_in", [NL_ALL, D, 2560], F32, kind="ExternalInput").ap()
    wout_d = dt("w_out", [NL_ALL, D, D], F32, kind="ExternalInput").ap()
    w1_d = dt("w1", [NL_ALL, D, 4096], F32, kind="ExternalInput").ap()
    w2_d = dt("w2", [NL_ALL, 4096, D], F32, kind="ExternalInput").ap()
    par_d = dt("par", [128, NPAR], F32, kind="ExternalInput").ap()
    bd_d = dt("bd", [NL_ALL, 128, 4, 128], F32, kind="ExternalInput").ap()
    c32_d = dt("c32", [128, 3, 128], F32, kind="ExternalInput").ap()
    cb_d = dt("cb", [128, 2, 128], BF16, kind="ExternalInput").ap()
    kc_d = dt("kconst", [2, 64, S], BF16, kind="ExternalInput").ap()
    qc_d = dt("qconst", [4, 64, S], BF16, kind="ExternalInput").ap()
    out_d = dt("out", [S, D], F32, kind="ExternalOutput").ap()

    st = ExitStack()
    with st:
        P = Prog(nc)
        sb = lambda name, shape, dty: st.enter_context(nc.sbuf_tensor("sb_" + name, shape, dty))
        xT = sb("xT", [128, 8, S], F32)
        hT = sb("hT", [128, 8, 1024], BF16)
        U32 = sb("U", [128, 8192], F32)
        Ub = U32.bitcast(BF16)
        YT = Ub[:, 0:8192].rearrange("p (c t) -> p c t", c=8)
        g0 = Ub[:, 8192:8192 + 2112].rearrange("p (c t) -> p c t", c=2)
        g1 = Ub[:, 10304:10304 + 2048].rearrange("p (c t) -> p c t", c=2)
        dg = Ub[:, 12352:12352 + 31 * 128].rearrange("p (k m) -> p k m", k=31)
        ytmp = U32[:, 4096:8192].rearrange("p (c t) -> p c t", c=8)
        macc = U32[:, :].rearrange("p (c t) -> p c t", c=8)
        kaug = [sb("kaug%d" % h, [128, S], BF16) for h in range(4)]
        qaug = [sb("qaug%d" % h, [128, 1024], BF16) for h in range(4)]
        vaug = sb("vaug", [128, 16, 384], BF16)
        NSTG, NWB = 2, 12
        stg = [sb("stg%d" % i, [128, 1024], F32) for i in range(NSTG)]
        wb = [sb("wb%d" % i, [128, 1024], BF16) for i in range(NWB)]
        par = sb("par", [128, NPAR], F32)
        gmask = par[:, NL_ALL * NPL:NPAR].rearrange("p (a b c) -> p a b c", a=4, b=2)
        c32 = sb("c32", [128, 3, 128], F32)
        cb = sb("cb", [128, 2, 128], BF16)
        onesb = sb("onesb", [128, 128], BF16)
        bdb = sb("bdb", [128, 4, 128], BF16)
        sq4 = sb("sq4", [128, 4, 512], BF16)
        rstd = sb("rstd", [128, 512], F32)
        NWK = 7
        wk = [sb("wk%d" % i, [128, 512], F32) for i in range(NWK)]
        wkb = [w.bitcast(BF16) for w in wk]
        ksum = sb("ksum", [128, 2, 8], F32)
        gt3 = sb("gt3", [128, 3, 8, 8], F32)
        selpad = sb("selpad", [128, 8, 72], BF16)
        lruc = sb("lruc", [128, 8], F32)
        hst = sb("hst", [128, 2], F32)
        tails = sb("tails", [128, 3, 2, 32], BF16)
        dummy = sb("dummy", [128, 4], F32)
        ident32, Sw32, Avg32 = c32[:, 0, :], c32[:, 1, :], c32[:, 2, :]
        identb, trib = cb[:, 0, :], cb[:, 1, :]
        psb = [st.enter_context(nc.psum_tensor("ps%d" % i, [128, 512], F32)) for i in range(8)]

        class Banks:
            def __init__(s, idx, name):
                s.idx, s.i, s.name = idx, 0, name

            def next(s):
                b = s.idx[s.i % len(s.idx)]
                s.i += 1
                return psb[b], ("ps", b)

        bk_mm = Banks([0, 1, 2], "mm")
        bk_sc = Banks([3, 4], "sc")
        bk_pv = Banks([5], "pv")
        bk_st = Banks([6], "st")
        bk_sm = Banks([7], "sm")
        wkc = [0]

        def work():
            i = wkc[0] % NWK
            wkc[0] += 1
            return i

        cnt = dict(stg=0, wb=0)

        def load_slice(src_ap, view3=None):
            slot = cnt["wb"] % NWB
            cnt["wb"] += 1
            o = wb[slot][:]
            if view3:
                o = o.rearrange("p (a b) -> p a b", a=view3)
            P.dma(lambda e: e.dma_start(out=o, in_=src_ap), w=[("wb", slot)], group="wb%d" % slot, eng="pool")
            return wb[slot], ("wb", slot)

        def kslice(w_d, l, c0):
            t, k = load_slice(w_d[l, :, c0:c0 + 128].rearrange("(kc p) c -> p kc c", p=128), view3=8)
            return t[:].rearrange("p (a b) -> p a b", a=8), k

        def pc(l, i):
            return par[:, l * NPL + i:l * NPL + i + 1]

        P.dma(lambda e: e.dma_start(out=par[:], in_=par_d), w=["par"], group="i0")
        P.dma(lambda e: e.dma_start(out=c32[:], in_=c32_d), w=["c32"], group="i1")
        P.dma(lambda e: e.dma_start(out=cb[:], in_=cb_d), w=["cb"], group="i2")
        for h in range(4):
            ty = h % 2
            rows = slice(64, 128) if ty == 0 else slice(0, 64)
            nr = 64
            P.dma(lambda e, h=h, rows=rows, nr=nr, ty=ty: e.dma_start(out=kaug[h][rows, :], in_=kc_d[ty, 0:nr, :]),
                  w=[("kaugc", h)], group="i3_%d" % h)
        P.pool(lambda e: e.memset(onesb[:], 1.0), w=["onesb"])
        P.pool(lambda e: e.memset(vaug[:, :, 64:128], 1.0), w=["vones"])
        P.pool(lambda e: e.memset(vaug[:, :, 256:320], 1.0), w=["vones"])
        P.pool(lambda e: e.memset(selpad[:], 0.0), w=["selpad"])
        P.pool(lambda e: e.memset(dummy[:], 0.0), w=["Uphase"])
        for ts in range(16):
            si = cnt["stg"] % NSTG
            cnt["stg"] += 1
            P.dma(lambda e, si=si, ts=ts: e.dma_start(out=stg[si][:], in_=x_d[ts * 128:(ts + 1) * 128, :]),
                  w=[("stg", si)], group="stg%d" % si)
            for g in range(2):
                pt, pk = bk_sm.next()
                for j in range(4):
                    dc = g * 4 + j
                    P.pe(lambda e, pt=pt, j=j, dc=dc, si=si: e.transpose(pt[:, j * 128:(j + 1) * 128], stg[si][:, dc * 128:(dc + 1) * 128], ident32),
                         r=[("stg", si), "c32"], w=[pk])
                P.act(lambda e, pt=pt, g=g, ts=ts: e.copy(out=xT[:, g * 4:(g + 1) * 4, ts * 128:(ts + 1) * 128],
                                                           in_=pt[:].rearrange("p (a b) -> p a b", a=4)),
                      r=[pk], w=[("xT", dc, ts // 4) for dc in range(g * 4, g * 4 + 4)])

        def barrier():
            P.dve(lambda e: e.memset(dummy[:, 0:1], 0.0), w=["Uphase"])

        UR = ["Uphase"]

        class StatAcc:
            def __init__(s_, pst, pstk, delay=3, slots=None):
                s_.pst, s_.pstk, s_.delay, s_.pend = pst, pstk, delay, []
                s_.slots = slots or [(sq4[:, j, :], ("sq4", j)) for j in range(4)]

            def add(s_, src_ap, rkeys, c):
                sl, slk = s_.slots[c % 4]
                P.act(lambda e, sl=sl: e.activation(out=sl, in_=src_ap, func=AF.Square), r=rkeys, w=[slk])
                s_.pend.append(c)
                while len(s_.pend) > s_.delay:
                    s_.mm(s_.pend.pop(0))

            def mm(s_, c):
                sl, slk = s_.slots[c % 4]
                pst, pstk = s_.pst, s_.pstk
                P.pe(lambda e, sl=sl, c=c, pst=pst: e.matmul(pst[:], lhsT=onesb[:], rhs=sl, start=(c == 0), stop=(c == 7)),
                     r=[slk, "onesb"], w=[pstk])

            def flush(s_):
                while s_.pend:
                    s_.mm(s_.pend.pop(0))

        def rstd_from(pst, pstk, scale, rs=None, rsk="rstd"):
            rs = rstd if rs is None else rs
            P.act(lambda e, pst=pst, rs=rs: e.activation(out=rs[:], in_=pst[:], func=AF.Ln, scale=scale, bias=1e-6), r=[pstk], w=[rsk])
            P.act(lambda e, rs=rs: e.activation(out=rs[:], in_=rs[:], func=AF.Exp, scale=-0.5), r=[rsk], w=[rsk])

        def rms_stats(src4_fn, rkeys_fn, scale):
            pst, pstk = bk_st.next()
            P.act(lambda e: e.activation(out=sq4[:], in_=src4_fn(0), func=AF.Square),
                  r=[k for c in range(0, 4) for k in rkeys_fn(c)], w=[("sq4", j) for j in range(4)])
            dsq = []
            for t2 in range(2):
                wi = work()
                dst = wkb[wi][:, :].rearrange("p (a b) -> p a b", a=2)
                srcv = src4_fn(1)[:, 2 * t2:2 * t2 + 2, :]
                P.dve(lambda e, dst=dst, srcv=srcv: e.tensor_tensor(out=dst, in0=srcv, in1=srcv, op=ALU.mult),
                      r=[k for c in range(4 + 2 * t2, 6 + 2 * t2) for k in rkeys_fn(c)], w=[("wk", wi)])
                dsq.append((dst, ("wk", wi)))
            for c in range(8):
                if c < 4:
                    rhs, rk = sq4[:, c, :], ("sq4", c)
                else:
                    dst, rk = dsq[(c - 4) // 2]
                    rhs = dst[:, (c - 4) % 2, :]
                P.pe(lambda e, c=c, pst=pst, rhs=rhs: e.matmul(pst[:], lhsT=onesb[:], rhs=rhs, start=(c == 0), stop=(c == 7)),
                     r=[rk, "onesb"], w=[pstk])
            rstd_from(pst, pstk, scale)
            return
            for g in range(2):
                P.act(lambda e, g=g: e.activation(out=sq4[:], in_=src4_fn(g), func=AF.Square),
                      r=[k for c in range(4 * g, 4 * g + 4) for k in rkeys_fn(c)], w=[("sq4", j) for j in range(4)])
                for j in range(4):
                    c = 4 * g + j
                    P.pe(lambda e, j=j, c=c, pst=pst: e.matmul(pst[:], lhsT=onesb[:], rhs=sq4[:, j, :], start=(c == 0), stop=(c == 7)),
                         r=[("sq4", j), "onesb"], w=[pstk])
            rstd_from(pst, pstk, scale)

        def apply_h(l, tt, gbase, rs=None, rsk="rstd"):
            lt = tt % 2
            rs = rstd if rs is None else rs
            for c in range(8):
                P.dve(lambda e, c=c, rs=rs: e.scalar_tensor_tensor(out=hT[:, c, lt * 512:(lt + 1) * 512], in0=xT[:, c, tt * 512:(tt + 1) * 512],
                                                                   scalar=pc(l, gbase + c), in1=rs[:], op0=ALU.mult, op1=ALU.mult),
                      r=[("xT", c, tt), rsk, "par"], w=[("hT", c, lt)])

        def update_x(l, tt, src, srckey_fn, gbase, next_stats=None):
            for dc in range(8):
                update_chunk(l, tt, src, srckey_fn, gbase, dc, next_stats)
            if next_stats is not None:
                next_stats.flush()

        def update_chunk(l, tt, src, srckey_fn, gbase, dc, next_stats=None):
            P.dve(lambda e, dc=dc: e.tensor_tensor(out=src(dc), in0=src(dc), in1=rstd[:], op=ALU.mult), r=[srckey_fn(dc), "rstd"] + UR, w=[srckey_fn(dc)])
            P.dve(lambda e, dc=dc: e.scalar_tensor_tensor(out=xT[:, dc, tt * 512:(tt + 1) * 512], in0=src(dc), scalar=pc(l, gbase + dc),
                                                          in1=xT[:, dc, tt * 512:(tt + 1) * 512], op0=ALU.mult, op1=ALU.add),
                  r=[srckey_fn(dc), ("xT", dc, tt), "par"] + UR, w=[("xT", dc, tt)])
            if next_stats is not None:
                next_stats.add(xT[:, dc, tt * 512:(tt + 1) * 512], [("xT", dc, tt)], dc)

        def norm_to_h(l, tt, gbase):
            rms_stats(lambda g: xT[:, 4 * g:4 * g + 4, tt * 512:(tt + 1) * 512], lambda c: [("xT", c, tt)], 1.0 / D)
            apply_h(l, tt, gbase)

        def proj(wt, wkey, tt):
            lt = tt % 2
            pt, pk = bk_mm.next()
            for kc in range(8):
                P.pe(lambda e, kc=kc, pt=pt: e.matmul(pt[:], lhsT=wt[:, kc, :], rhs=hT[:, kc, lt * 512:(lt + 1) * 512], start=(kc == 0), stop=(kc == 7)),
                     r=[wkey, ("hT", kc, lt)], w=[pk])
            return pt, pk

        def build_dg(l, base, ntap, c):
            for k in range(ntap):
                P.dve(lambda e, k=k: e.tensor_scalar(out=dg[:, k, :], in0=identb, scalar1=pc(l, base + k * 2 + c), scalar2=None, op0=ALU.mult),
                      r=["cb", "par"] + UR, w=[("dg", k)])

        def conv(ntap, c, tt, grp):
            lt = tt % 2
            pt, pk = bk_mm.next()
            for k in range(ntap):
                off = 32 - (ntap - 1) + k + lt * 512
                P.pe(lambda e, k=k, off=off, pt=pt: e.matmul(pt[:], lhsT=dg[:, k, :], rhs=g0[:, c, off:off + 512], start=(k == 0), stop=(k == ntap - 1)),
                     r=[("dg", k), ("g0", c, lt), ("g0", c, lt - 1)] + UR, w=[pk])
            return pt, pk

        def pad_in(hf, c, grp):
            if hf == 0:
                P.dve(lambda e: e.memset(g0[:, c, 0:32], 0.0), r=UR, w=[("g0", c, -1)])
            else:
                P.dve(lambda e: e.tensor_copy(out=g0[:, c, 0:32], in_=tails[:, grp, c, :]), r=[("tails", grp, c)] + UR, w=[("g0", c, -1)])

        def pad_out(hf, c, grp):
            if hf == 0:
                P.dve(lambda e: e.tensor_copy(out=tails[:, grp, c, :], in_=g0[:, c, 1024:1056]), r=[("g0", c, 1)] + UR, w=[("tails", grp, c)])

        for l in range(NL):
            P.epoch = l
            if on(0.1):
                P.act(lambda e, l=l: e.activation(out=lruc[:, 0:2], in_=par[:, l * NPL + 44:l * NPL + 46], func=AF.Exp, scale=-1.0), r=["par"], w=["lruc0"])
                P.act(lambda e: e.activation(out=lruc[:, 2:4], in_=lruc[:, 0:2], func=AF.Ln, bias=1.0), r=["lruc0"], w=["lruc1"])
                P.dve(lambda e: e.tensor_scalar(out=lruc[:, 4:6], in0=lruc[:, 2:4], scalar1=-8.0, scalar2=None, op0=ALU.mult), r=["lruc1"], w=["lruc2"])
                P.dve(lambda e: e.tensor_scalar(out=lruc[:, 6:8], in0=lruc[:, 2:4], scalar1=-16.0, scalar2=None, op0=ALU.mult), r=["lruc1"], w=["lruc3"])
                si = cnt["stg"] % NSTG
                cnt["stg"] += 1
                P.dma(lambda e, si=si, l=l: e.dma_start(out=stg[si][:, 0:512].rearrange("p (a b) -> p a b", a=4), in_=bd_d[l]), w=[("stg", si)], group="stg%d" % si)
                P.dve(lambda e, si=si: e.tensor_copy(out=bdb[:].rearrange("p a b -> p (a b)"), in_=stg[si][:, 0:512]), r=[("stg", si)], w=["bdb"])

            for hf in range(2):
                tts = [2 * hf, 2 * hf + 1]
                barrier()
                for h in range(4 * on(0.5)):
                    ty = h % 2
                    rows = slice(64, 128) if ty == 0 else slice(0, 64)
                    nr = 64
                    P.dma(lambda e, h=h, rows=rows, nr=nr, hf=hf: e.dma_start(out=qaug[h][rows, :], in_=qc_d[h, 0:nr, hf * 1024:(hf + 1) * 1024]),
                          w=[("qaugc", h)], group="qc%d" % h)
                kws = [kslice(win_d, l, 1536 + c2 * 128) for c2 in range(2)]
                for tt in tts:
                    norm_to_h(l, tt, 0)
                    for c2 in range(2):
                        wt, wkey = kws[c2]
                        pt, pk = proj(wt, wkey, tt)
                        P.act(lambda e, pt=pt, c2=c2, tt=tt: e.copy(out=kaug[2 * c2][0:64, tt * 512:(tt + 1) * 512], in_=pt[0:64, :]), r=[pk], w=[("kaug", 2 * c2, tt)])
                        P.act(lambda e, pt=pt, c2=c2, tt=tt: e.copy(out=kaug[2 * c2 + 1][64:128, tt * 512:(tt + 1) * 512], in_=pt[64:128, :]), r=[pk], w=[("kaug", 2 * c2 + 1, tt)])
                        P.dve(lambda e, pt=pt, c2=c2, tt=tt: e.tensor_reduce(out=ksum[:, c2, 2 * tt:2 * tt + 2], in_=pt[:].rearrange("p (a b) -> p a b", a=2), axis=AX.X, op=ALU.add),
                              r=[pk], w=[("ksum", c2, tt)])
                vs = [kslice(win_d, l, 1792 + j * 128) for j in range(2 * on(2))]
                for ts in range(8 * hf, 8 * hf + 8 * on(2)):
                    lts = ts - 8 * hf
                    pt, pk = bk_mm.next()
                    for j in range(2):
                        wt, wkey = vs[j]
                        for kc in range(8):
                            P.pe(lambda e, pt=pt, j=j, kc=kc, wt=wt, lts=lts: e.matmul(pt[:, j * 128:(j + 1) * 128], lhsT=hT[:, kc, lts * 128:(lts + 1) * 128], rhs=wt[:, kc, :],
                                                                                       start=(kc == 0), stop=(kc == 7)),
                                 r=[wkey, ("hT", kc, lts // 4)], w=[pk])
                    vv = vaug[:, ts, :].rearrange("p (a b c) -> p a b c", a=2, b=3)
                    for pr in range(2):
                        for od in range(2):
                            P.act(lambda e, pt=pt, pr=pr, od=od, vv=vv: e.copy(out=vv[:, pr, 2 * od, :], in_=pt[:, (2 * pr + od) * 64:(2 * pr + od + 1) * 64]),
                                  r=[pk], w=[("vaug", ts)])
                qs = [kslice(win_d, l, 1280 + j * 128) for j in range(2 * on(3))]
                for tt in tts[:2 * on(3)]:
                    lt = tt % 2
                    for c2 in range(2):
                        wt, wkey = qs[c2]
                        pt, pk = proj(wt, wkey, tt)
                        hA, hB = 2 * c2, 2 * c2 + 1
                        P.act(lambda e, pt=pt, hA=hA, lt=lt: e.activation(out=qaug[hA][0:64, lt * 512:(lt + 1) * 512], in_=pt[0:64, :], func=AF.Identity, scale=0.125, bias=0.0),
                              r=[pk], w=[("qaug", hA, lt)])
                        P.act(lambda e, pt=pt, hB=hB, lt=lt: e.activation(out=qaug[hB][64:128, lt * 512:(lt + 1) * 512], in_=pt[64:128, :], func=AF.Identity, scale=0.125, bias=0.0),
                              r=[pk], w=[("qaug", hB, lt)])
                        if tt >= 2:
                            qi = work()
                            q32 = wk[qi]
                            P.act(lambda e, pt=pt, q32=q32: e.copy(out=q32[:], in_=pt[:]), r=[pk], w=[("wk", qi)])
                            gps, gpsk = bk_sm.next()
                            for sub in range(4):
                                for ty in range(2):
                                    i8 = sub * 2 + ty
                                    rows = slice(0, 64) if ty == 0 else slice(64, 128)
                                    P.pe(lambda e, gps=gps, q32=q32, rows=rows, sub=sub, c2=c2, i8=i8: e.matmul(gps[:, i8 * 8:(i8 + 1) * 8], lhsT=q32[rows, sub * 128:(sub + 1) * 128], rhs=ksum[rows, c2, :], start=True, stop=True),
                                         r=[("wk", qi)] + [("ksum", c2, t2) for t2 in range(4)], w=[gpsk])
                            tps = [(psb[6], ("ps", 6)), (psb[5], ("ps", 5))]
                            for sub in range(4):
                                own = (tt * 512 + sub * 128) // 256
                                for ty in range(2):
                                    i8 = sub * 2 + ty
                                    off = 64 if ty == 0 else 32
                                    gk = ("gt", i8)
                                    P.dve(lambda e, gps=gps, i8=i8, own=own: e.tensor_tensor(out=gt3[:, 0, i8, :], in0=gps[:, i8 * 8:(i8 + 1) * 8], in1=gmask[:, own - 4, 0, :], op=ALU.add),
                                          r=[gpsk, "par"], w=[gk])
                                    P.dve(lambda e, gps=gps, i8=i8, own=own: e.tensor_tensor(out=gt3[:, 1, i8, :], in0=gps[:, i8 * 8:(i8 + 1) * 8], in1=gmask[:, own - 4, 1, :], op=ALU.add),
                                          r=[gpsk, "par"], w=[gk])
                                    P.dve(lambda e, i8=i8: e.max(out=gt3[:, 2, i8, :], in_=gt3[:, 0, i8, :]), r=[gk], w=[gk])
                                    P.dve(lambda e, i8=i8, off=off: e.tensor_scalar(out=selpad[:, i8, off:off + 8], in0=gt3[:, 1, i8, :], scalar1=gt3[:, 2, i8, 2:3], scalar2=-BIG, op0=ALU.is_lt, op1=ALU.mult),
                                          r=[gk], w=[("selpad", i8)])
                                    tp, tpk = tps[ty]
                                    P.pe(lambda e, tp=tp, off=off, i8=i8, sub=sub: e.matmul(tp[0:off + 8, sub * 128:(sub + 1) * 128], lhsT=selpad[:, i8, 0:off + 8], rhs=identb, start=True, stop=True),
                                         r=[("selpad", i8), "cb"], w=[tpk])
                            for ty in range(2):
                                h = 2 * c2 + ty
                                off = 64 if ty == 0 else 32
                                tp, tpk = tps[ty]
                                P.act(lambda e, tp=tp, off=off, h=h, lt=lt: e.copy(out=qaug[h][off:off + 8, lt * 512:(lt + 1) * 512], in_=tp[off:off + 8, :]),
                                      r=[tpk, ("qaugc", h)], w=[("qaug", h, lt)])
                    for h in range(4 * on(4)):
                        ty = h % 2
                        rows = slice(0, 128)
                        nrows = slice(0, 64) if ty == 0 else slice(64, 128)
                        pr = h // 2
                        vc0 = pr * 192 + (0 if ty == 0 else 64)
                        nkt = (tt + 1) * 4
                        pv, pvk = bk_pv.next()
                        def emit_sc(kt, h=h, tt=tt, lt=lt):
                            o = kt - tt * 4
                            n0 = max(o, 0) * 128
                            sc, sck = bk_sc.next()
                            P.pe(lambda e, sc=sc, h=h, kt=kt, n0=n0, o=o, lt=lt: e.matmul(sc[:, n0:512], lhsT=kaug[h][:, kt * 128:(kt + 1) * 128],
                                                                                     rhs=qaug[h][:, lt * 512 + n0:(lt + 1) * 512], start=True, stop=(o < 0)),
                                 r=[("kaug", h, kt // 4), ("kaugc", h), ("qaug", h, lt), ("qaugc", h)], w=[sck])
                            if o >= 0:
                                P.pe(lambda e, sc=sc, n0=n0: e.matmul(sc[:, n0:n0 + 128], lhsT=identb, rhs=trib, start=False, stop=True), r=["cb"], w=[sck])
                            pi = work()
                            PT = wkb[pi][:, 0:512]
                            P.act(lambda e, sc=sc, PT=PT, n0=n0: e.activation(out=PT[:, n0:512], in_=sc[:, n0:512], func=AF.Exp), r=[sck], w=[("wk", pi)])
                            return PT, pi, n0

                        def emit_pv(kt, info, pv=pv, pvk=pvk, vc0=vc0, nkt=nkt):
                            PT, pi, n0 = info
                            P.pe(lambda e, pv=pv, PT=PT, n0=n0, kt=kt, vc0=vc0, nkt=nkt: e.matmul(pv[:, n0:512], lhsT=vaug[:, kt, vc0:vc0 + 128], rhs=PT[:, n0:512],
                                                                                                start=(kt == 0), stop=(kt == nkt - 1)),
                                 r=[("wk", pi), ("vaug", kt), "vones"], w=[pvk])

                        infos = {0: emit_sc(0)}
                        for kt in range(nkt):
                            if kt + 1 < nkt:
                                infos[kt + 1] = emit_sc(kt + 1)
                            emit_pv(kt, infos.pop(kt))
                        si_ = work()
                        pvs = wk[si_]
                        P.dve(lambda e, pv=pv, pvs=pvs: e.tensor_copy(out=pvs[:], in_=pv[:]), r=[pvk], w=[("wk", si_)])
                        pst, pstk = bk_st.next()
                        P.pe(lambda e, pst=pst, pvs=pvs: e.matmul(pst[:], lhsT=Sw32, rhs=pvs[:], start=True, stop=True), r=[("wk", si_), "c32"], w=[pstk])
                        ri = work()
                        rec = wk[ri]
                        P.act(lambda e, pst=pst, rec=rec, nrows=nrows: e.activation(out=rec[nrows, :], in_=pst[nrows, :], func=AF.Ln), r=[pstk], w=[("wk", ri)])
                        P.act(lambda e, rec=rec, nrows=nrows: e.activation(out=rec[nrows, :], in_=rec[nrows, :], func=AF.Exp, scale=-1.0), r=[("wk", ri)], w=[("wk", ri)])
                        P.dve(lambda e, pvs=pvs, rec=rec, nrows=nrows, pr=pr, lt=lt: e.tensor_tensor(out=YT[nrows, 4 + pr, lt * 512:(lt + 1) * 512], in0=pvs[nrows, :], in1=rec[nrows, :], op=ALU.mult),
                              r=[("wk", si_), ("wk", ri)] + UR, w=[("YT", 4 + pr, lt, ty)])

                for c in range(2 * on(5)):
                    build_dg(l, 46, 31, c)
                    pad_in(hf, c, 0)
                    wv_, wvk = kslice(win_d, l, c * 128)
                    wg_, wgk = kslice(win_d, l, 256 + c * 128)
                    for tt in tts:
                        lt = tt % 2
                        pv_, pvk_ = proj(wv_, wvk, tt)
                        pg_, pgk_ = proj(wg_, wgk, tt)
                        si_ = work()
                        sg = wk[si_]
                        P.act(lambda e, pg_=pg_, sg=sg: e.activation(out=sg[:], in_=pg_[:], func=AF.Sigmoid), r=[pgk_], w=[("wk", si_)])
                        P.dve(lambda e, pv_=pv_, sg=sg, c=c, lt=lt: e.tensor_tensor(out=g0[:, c, 32 + lt * 512:32 + (lt + 1) * 512], in0=pv_[:], in1=sg[:], op=ALU.mult),
                              r=[pvk_, ("wk", si_)] + UR, w=[("g0", c, lt)])
                    pad_out(hf, c, 0)
                    bk_gn = Banks([3, 4, 5, 6], "gn")
                    ZC = {}
                    for tt in tts:
                        pt, pk = conv(31, c, tt, 0)
                        zi = work()
                        zc = wk[zi]
                        P.act(lambda e, l=l, pt=pt, zc=zc, c=c: e.activation(out=zc[:], in_=pt[:], func=AF.Identity, bias=pc(l, 32 + c), scale=1.0), r=[pk, "par"], w=[("wk", zi)])
                        ZC[tt] = (zc, zi)
                    SQ = {}
                    for tt in tts:
                        zc, zi = ZC[tt]
                        pst, pstk = bk_gn.next()
                        P.pe(lambda e, pst=pst, zc=zc: e.matmul(pst[:], lhsT=Avg32, rhs=zc[:], start=True, stop=True), r=[("wk", zi), "c32"], w=[pstk])
                        P.dve(lambda e, pst=pst, zc=zc: e.tensor_tensor(out=zc[:], in0=zc[:], in1=pst[:], op=ALU.subtract), r=[("wk", zi), pstk], w=[("wk", zi)])
                        qi = work()
                        s32 = wk[qi]
                        P.act(lambda e, zc=zc, s32=s32: e.activation(out=s32[:], in_=zc[:], func=AF.Square), r=[("wk", zi)], w=[("wk", qi)])
                        SQ[tt] = (s32, qi)
                    for tt in tts:
                        lt = tt % 2
                        zc, zi = ZC[tt]
                        s32, qi = SQ[tt]
                        pst2, pstk2 = bk_gn.next()
                        P.pe(lambda e, pst2=pst2, s32=s32: e.matmul(pst2[:], lhsT=Avg32, rhs=s32[:], start=True, stop=True), r=[("wk", qi), "c32"], w=[pstk2])
                        P.act(lambda e, pst2=pst2, s32=s32: e.activation(out=s32[:], in_=pst2[:], func=AF.Ln, bias=1e-5, scale=1.0), r=[pstk2], w=[("wk", qi)])
                        P.act(lambda e, s32=s32: e.activation(out=s32[:], in_=s32[:], func=AF.Exp, scale=-0.5), r=[("wk", qi)], w=[("wk", qi)])
                        P.dve(lambda e, zc=zc, s32=s32: e.tensor_tensor(out=zc[:], in0=zc[:], in1=s32[:], op=ALU.mult), r=[("wk", zi), ("wk", qi)], w=[("wk", zi)])
                        P.act(lambda e, l=l, zc=zc, c=c, lt=lt: e.activation(out=YT[:, c, lt * 512:(lt + 1) * 512], in_=zc[:], func=AF.Silu, scale=pc(l, 34 + c), bias=pc(l, 36 + c)),
                              r=[("wk", zi), "par"] + UR, w=[("YT", c, lt)])

                for c in range(2 * on(6)):
                    build_dg(l, 108, 3, c)
                    pad_in(hf, c, 1)
                    wb_, wbk = kslice(win_d, l, 512 + c * 128)
                    wc_, wck = kslice(win_d, l, 768 + c * 128)
                    wx_, wxk = kslice(win_d, l, 1024 + c * 128)
                    for tt in tts:
                        lt = tt % 2
                        px_, pxk_ = proj(wx_, wxk, tt)
                        xi = work()
                        xs32 = wk[xi]
                        P.act(lambda e, px_=px_, xs32=xs32: e.copy(out=xs32[:], in_=px_[:]), r=[pxk_], w=[("wk", xi)])
                        pc_, pck_ = proj(wc_, wck, tt)
                        P.dve(lambda e, pc_=pc_, xs32=xs32, c=c, lt=lt: e.tensor_tensor(out=g0[:, c, 32 + lt * 512:32 + (lt + 1) * 512], in0=pc_[:], in1=xs32[:], op=ALU.mult),
                              r=[pck_, ("wk", xi)] + UR, w=[("g0", c, lt)])
                        pb_, pbk_ = proj(wb_, wbk, tt)
                        P.act(lambda e, pb_=pb_, c=c, lt=lt: e.copy(out=g1[:, c, lt * 512:(lt + 1) * 512], in_=pb_[:]), r=[pbk_] + UR, w=[("g1", c, lt)])
                    pad_out(hf, c, 1)
                    for tt in tts:
                        lt = tt % 2
                        pt, pk = conv(3, c, tt, 1)
                        P.dve(lambda e, pt=pt, c=c, lt=lt: e.tensor_tensor(out=YT[:, 2 + c, lt * 512:(lt + 1) * 512], in0=pt[:], in1=g1[:, c, lt * 512:(lt + 1) * 512], op=ALU.mult),
                              r=[pk, ("g1", c, lt)] + UR, w=[("YT", 2 + c, lt)])

                for c in range(2 * on(7)):
                    build_dg(l, 114, 4, c)
                    pad_in(hf, c, 2)
                    wr_, wrk = kslice(win_d, l, 2048 + c * 128)
                    wg_, wgk = kslice(win_d, l, 2304 + c * 128)
                    for tt in tts:
                        lt = tt % 2
                        pr_, prk_ = proj(wr_, wrk, tt)
                        P.act(lambda e, pr_=pr_, c=c, lt=lt: e.copy(out=g0[:, c, 32 + lt * 512:32 + (lt + 1) * 512], in_=pr_[:]), r=[prk_] + UR, w=[("g0", c, lt)])
                        pg_, pgk_ = proj(wg_, wgk, tt)
                        P.act(lambda e, pg_=pg_, c=c, lt=lt: e.activation(out=g1[:, c, lt * 512:(lt + 1) * 512], in_=pg_[:], func=AF.Gelu_apprx_tanh), r=[pgk_] + UR, w=[("g1", c, lt)])
                    pad_out(hf, c, 2)
                    bk_bd = Banks([3, 4, 5, 6], "bd")
                    XC = {}
                    for n_, tt in enumerate(tts):
                        pt, pk = conv(4, c, tt, 2)
                        xi = n_
                        xc32 = wk[xi]
                        xcb = wkb[2][:, n_ * 512:(n_ + 1) * 512]
                        P.act(lambda e, l=l, pt=pt, xc32=xc32, c=c: e.activation(out=xc32[:], in_=pt[:], func=AF.Identity, bias=pc(l, 38 + c), scale=1.0), r=[pk, "par"], w=[("wk", xi)])
                        P.act(lambda e, l=l, pt=pt, xcb=xcb, c=c: e.activation(out=xcb, in_=pt[:], func=AF.Identity, bias=pc(l, 38 + c), scale=1.0), r=[pk, "par"], w=[("wk", 2)])
                        XC[tt] = (xc32, xi, xcb)
                    PA = {}
                    for tt in tts:
                        xc32, xi, xcb = XC[tt]
                        pa, pak = bk_bd.next()
                        P.pe(lambda e, pa=pa, xcb=xcb, c=c: e.matmul(pa[:], lhsT=bdb[:, c, :], rhs=xcb, start=True, stop=True), r=[("wk", 2), "bdb"], w=[pak])
                        px, pxk = bk_bd.next()
                        P.pe(lambda e, px=px, xcb=xcb, c=c: e.matmul(px[:], lhsT=bdb[:, 2 + c, :], rhs=xcb, start=True, stop=True), r=[("wk", 2), "bdb"], w=[pxk])
                        PA[tt] = (pa, pak, px, pxk)
                    for n_, tt in enumerate(tts):
                        lt = tt % 2
                        xc32, xi, xcb = XC[tt]
                        pa, pak, px, pxk = PA[tt]
                        r_i, i_i, e_i = (3, 4, 5) if n_ == 0 else (6, 3, 4)
                        rt, it, et = wk[r_i], wk[i_i], wk[e_i]
                        P.act(lambda e, l=l, pa=pa, rt=rt, c=c: e.activation(out=rt[:], in_=pa[:], func=AF.Sigmoid, bias=pc(l, 40 + c), scale=1.0), r=[pak, "par"], w=[("wk", r_i)])
                        P.act(lambda e, l=l, px=px, it=it, c=c: e.activation(out=it[:], in_=px[:], func=AF.Sigmoid, bias=pc(l, 42 + c), scale=1.0), r=[pxk, "par"], w=[("wk", i_i)])
                        P.act(lambda e, rt=rt, et=et, c=c: e.activation(out=et[:], in_=rt[:], func=AF.Exp, scale=lruc[:, 6 + c:7 + c]), r=[("wk", r_i), "lruc3"], w=[("wk", e_i)])
                        P.act(lambda e, rt=rt, c=c: e.activation(out=rt[:], in_=rt[:], func=AF.Exp, scale=lruc[:, 4 + c:5 + c]), r=[("wk", r_i), "lruc2", ("wk", e_i)], w=[("wk", r_i)])
                        P.act(lambda e, et=et: e.activation(out=et[:], in_=et[:], func=AF.Sqrt, scale=-1.0, bias=1.0), r=[("wk", e_i)], w=[("wk", e_i)])
                        P.dve(lambda e, it=it, xc32=xc32: e.tensor_tensor(out=it[:], in0=it[:], in1=xc32[:], op=ALU.mult), r=[("wk", i_i), ("wk", xi)], w=[("wk", i_i)])
                        P.dve(lambda e, it=it, et=et: e.tensor_tensor(out=it[:], in0=it[:], in1=et[:], op=ALU.mult), r=[("wk", i_i), ("wk", e_i)], w=[("wk", i_i)])
                        init = 0.0 if tt == 0 else hst[:, c:c + 1]
                        P.dve(lambda e, et=et, rt=rt, it=it, init=init: e.tensor_tensor_scan(out=et[:], data0=rt[:], data1=it[:], initial=init, op0=ALU.mult, op1=ALU.add),
                              r=[("wk", r_i), ("wk", i_i), ("hst", c)], w=[("wk", e_i)])
                        P.dve(lambda e, et=et, c=c: e.tensor_copy(out=hst[:, c:c + 1], in_=et[:, 511:512]), r=[("wk", e_i)], w=[("hst", c)])
                        P.dve(lambda e, et=et, c=c, lt=lt: e.tensor_tensor(out=YT[:, 6 + c, lt * 512:(lt + 1) * 512], in0=et[:], in1=g1[:, c, lt * 512:(lt + 1) * 512], op=ALU.mult),
                              r=[("wk", e_i), ("g1", c, lt)] + UR, w=[("YT", 6 + c, lt)])

                bk_y = Banks([3, 4, 5], "y")
                w1c, w2c = {}, {}
                mstat = {}

                def W1(fg, l=l):
                    if fg not in w1c:
                        w1c[fg] = [kslice(w1_d, l, fg * 512 + j * 128) for j in range(4)]
                    return w1c[fg]

                def W2(fg, l=l):
                    if fg not in w2c:
                        w2c[fg] = [load_slice(w2_d[l, fg * 512 + j * 128:fg * 512 + (j + 1) * 128, :]) for j in range(4)]
                    return w2c[fg]

                def emit_hid(fg, tt):
                    hid = []
                    hi = None
                    for j in range(4):
                        wt, wkey = W1(fg)[j]
                        pt, pk = proj(wt, wkey, tt)
                        if j % 2 == 0:
                            hi = work()
                        hd = wkb[hi][:, (j % 2) * 512:(j % 2 + 1) * 512]
                        hk = ("wk", hi)
                        P.act(lambda e, pt=pt, hd=hd: e.activation(out=hd, in_=pt[:], func=AF.Relu), r=[pk], w=[hk])
                        P.dve(lambda e, hd=hd, pt=pt: e.tensor_tensor(out=hd, in0=hd, in1=pt[:], op=ALU.mult), r=[hk, pk], w=[hk])
                        hid.append((hd, hk, hi))
                    return hid

                def emit_y(fg, tt, hid, hook=None):
                    lt = tt % 2
                    for dc in range(8):
                        pt, pk = bk_y.next()
                        for j in range(4):
                            wt, wkey = W2(fg)[j]
                            hd, hk, hi = hid[j]
                            P.pe(lambda e, pt=pt, wt=wt, hd=hd, dc=dc, j=j: e.matmul(pt[:], lhsT=wt[:, dc * 128:(dc + 1) * 128], rhs=hd, start=(j == 0), stop=(j == 3)),
                                 r=[wkey, hk], w=[pk])
                        mo = macc[:, dc, lt * 512:(lt + 1) * 512]
                        if fg == 0:
                            P.act(lambda e, pt=pt, mo=mo: e.copy(out=mo, in_=pt[:]), r=[pk] + UR, w=[("macc", dc, lt)])
                        else:
                            P.dve(lambda e, pt=pt, mo=mo: e.tensor_tensor(out=mo, in0=mo, in1=pt[:], op=ALU.add), r=[pk, ("macc", dc, lt)] + UR, w=[("macc", dc, lt)])
                        if fg == 7:
                            if dc == 0:
                                mstat[lt] = StatAcc(psb[6 + lt], ("ps", 6 + lt))
                            mstat[lt].add(mo, [("macc", dc, lt)] + UR, dc)
                            if dc == 7:
                                mstat[lt].flush()
                        if hook is not None:
                            hook(dc)

                barrier()
                wos = [kslice(wout_d, l, dc * 128) for dc in range(8 * on(8))]
                ytk = lambda c: [("YT", c, lt)] if c not in (4, 5) else [("YT", c, lt, 0), ("YT", c, lt, 1)]
                if on(8):
                    pstA, pstAk = psb[6], ("ps", 6)
                    pstB, pstBk = psb[7], ("ps", 7)
                    sbt = [work(), work()]
                    slotsB = [(wkb[sbt[j // 2]][:, (j % 2) * 512:(j % 2 + 1) * 512], ("wk", sbt[j // 2])) for j in range(4)]
                    rs2i = work()
                    rs2, rs2k = wk[rs2i], ("wk", rs2i)
                    ysrc = lambda dc: ytmp[:, dc, :]
                    ykey = lambda dc: ("ytmp", dc)

                    def wout_group(tt, dc, stA):
                        lt = tt % 2
                        wt, wkey = wos[dc]
                        pt, pk = bk_mm.next()
                        ytk_ = lambda c: [("YT", c, lt)] if c not in (4, 5) else [("YT", c, lt, 0), ("YT", c, lt, 1)]
                        for c in range(8):
                            P.pe(lambda e, pt=pt, wt=wt, c=c, lt=lt: e.matmul(pt[:], lhsT=wt[:, c, :], rhs=YT[:, c, lt * 512:(lt + 1) * 512], start=(c == 0), stop=(c == 7)),
                                 r=[wkey] + ytk_(c) + UR, w=[pk])
                        P.act(lambda e, pt=pt, dc=dc: e.copy(out=ytmp[:, dc, :], in_=pt[:]), r=[pk] + UR, w=[("ytmp", dc)])
                        stA.add(pt[:], [pk], dc)

                    tt0, tt1 = tts
                    stA0 = StatAcc(pstA, pstAk)
                    for dc in range(8):
                        wout_group(tt0, dc, stA0)
                    stA0.flush()
                    rstd_from(pstA, pstAk, 1.0 / D)
                    stB0 = StatAcc(pstB, pstBk, slots=slotsB)
                    stA1 = StatAcc(pstA, pstAk)
                    for dc in range(8):
                        update_chunk(l, tt0, ysrc, ykey, 8, dc, stB0)
                        wout_group(tt1, dc, stA1)
                    stB0.flush()
                    stA1.flush()
                    rstd_from(pstB, pstBk, 1.0 / D, rs2, rs2k)
                    apply_h(l, tt0, 16, rs2, rs2k)
                    rstd_from(pstA, pstAk, 1.0 / D)
                    hid0_pre = emit_hid(0, tt0) if on(9) else None
                    stB1 = StatAcc(pstB, pstBk, slots=slotsB)
                    for dc in range(8):
                        update_chunk(l, tt1, ysrc, ykey, 8, dc, stB1)
                    stB1.flush()
                    rstd_from(pstB, pstBk, 1.0 / D, rs2, rs2k)
                    apply_h(l, tt1, 16, rs2, rs2k)

                barrier()
                its = [(fg, tt) for fg in range(8 * on(9)) for tt in tts]
                hids = {}
                if its:
                    hids[0] = hid0_pre
                for n, (fg, tt) in enumerate(its):
                    if n + 1 < len(its):
                        hids[n + 1] = emit_hid(*its[n + 1])
                    hook = None
                    if fg == 7 and tt == tts[1]:
                        rstd_from(psb[6], ("ps", 6), 1.0 / D)
                        hook = lambda dc, l=l, t0=tts[0]: update_chunk(l, t0, lambda d_: macc[:, d_, 0:512], lambda d_: ("macc", d_, 0), 24, dc)
                    emit_y(fg, tt, hids.pop(n), hook)
                for tt in tts[1:2 * on(9)]:
                    lt = tt % 2
                    rstd_from(psb[6 + lt], ("ps", 6 + lt), 1.0 / D)
                    update_x(l, tt, lambda dc, lt=lt: macc[:, dc, lt * 512:(lt + 1) * 512], lambda dc, lt=lt: ("macc", dc, lt), 24)

        for ts in range(16):
            si = cnt["stg"] % NSTG
            cnt["stg"] += 1
            for g in range(2):
                pt, pk = bk_sm.next()
                for j in range(4):
                    dc = g * 4 + j
                    P.pe(lambda e, pt=pt, j=j, dc=dc, ts=ts: e.transpose(pt[:, j * 128:(j + 1) * 128], xT[:, dc, ts * 128:(ts + 1) * 128], ident32),
                         r=[("xT", dc, ts // 4), "c32"], w=[pk])
                P.act(lambda e, pt=pt, g=g, si=si: e.copy(out=stg[si][:, g * 512:(g + 1) * 512], in_=pt[:]), r=[pk], w=[("stg", si)])
            P.dma(lambda e, si=si, ts=ts: e.dma_start(out=out_d[ts * 128:(ts + 1) * 128, :], in_=stg[si][:]), r=[("stg", si)], group="stg%d" % si)
        P.build(st)
        print("ops", len(P.ops), "sems", P.nsem)
    return nc


def _prep_consts():
    import ml_dtypes
    bf = ml_dtypes.bfloat16
    ident = np.eye(128, dtype=np.float32)
    sw = np.zeros((128, 128), np.float32)
    for i in range(128):
        sw[i, (i + 64) % 128] = 1.0
    avg = np.zeros((128, 128), np.float32)
    avg[0:64, 0:64] = 1.0 / 64
    avg[64:128, 64:128] = 1.0 / 64
    c32 = np.ascontiguousarray(np.stack([ident, sw, avg], axis=1))
    ii, jj = np.meshgrid(np.arange(128), np.arange(128), indexing="ij")
    tri = np.where(ii <= jj, 0.0, -BIG).astype(np.float32)
    cb = np.ascontiguousarray(np.stack([ident, tri], axis=1)).astype(bf)
    s = np.arange(S)
    kconst = np.zeros((2, 64, S), np.float32)
    qconst = np.zeros((4, 64, S), np.float32)
    for ty in range(2):
        b = 0 if ty == 0 else 32
        for j in range(8):
            kconst[ty, b + j] = (s // 256 == j)
        kconst[ty, b + 8] = 1.0
        kconst[ty, b + 9] = 1.0
        kconst[ty, b + 10] = 256.0 * (s // 256)
        kconst[ty, b + 11] = s % 256
    for h in range(4):
        b = 0 if h % 2 == 0 else 32
        sl = SLOPES[h]
        qconst[h, b + 8] = -sl * 256.0 * (s // 256)
        qconst[h, b + 9] = -sl * (s % 256)
        qconst[h, b + 10] = sl
        qconst[h, b + 11] = sl
    return c32, cb, kconst.astype(bf), qconst.astype(bf)


def _prep_inputs(inp):
    f = lambda k: np.asarray(inp[k], dtype=np.float32)
    par = np.zeros((128, NPAR), np.float32)
    gm = np.zeros((4, 2, 8), np.float32)
    for o in range(4):
        gm[o, 0, o + 4:] = -3.0e38
        gm[o, 1, o + 4:] = 3.0e38
    par[:, NL_ALL * NPL:] = gm.reshape(1, 64)
    v8 = lambda v: v.reshape(8, 128).T
    v2 = lambda v: v.reshape(2, 128).T
    cw = lambda w: w.reshape(w.shape[0], 2, 128).transpose(2, 0, 1).reshape(128, w.shape[0] * 2)
    for l in range(NL_ALL):
        b = l * NPL
        par[:, b + 0:b + 8] = v8(f("pre_mix_g")[l])
        par[:, b + 8:b + 16] = v8(f("post_mix_g")[l])
        par[:, b + 16:b + 24] = v8(f("pre_mlp_g")[l])
        par[:, b + 24:b + 32] = v8(f("post_mlp_g")[l])
        par[:, b + 32:b + 34] = v2(f("conf_dw_b")[l])
        par[:, b + 34:b + 36] = v2(f("conf_gn_g")[l])
        par[:, b + 36:b + 38] = v2(f("conf_gn_b")[l])
        par[:, b + 38:b + 40] = v2(f("lru_conv_b")[l])
        par[:, b + 40:b + 42] = v2(f("lru_ba")[l])
        par[:, b + 42:b + 44] = v2(f("lru_bx")[l])
        par[:, b + 44:b + 46] = v2(f("lru_lam")[l])
        par[:, b + 46:b + 108] = cw(f("conf_dw_w")[l])
        par[:, b + 108:b + 114] = cw(f("sconv_w")[l])
        par[:, b + 114:b + 122] = cw(f("lru_conv_w")[l])
    bd = np.zeros((NL_ALL, 128, 4, 128), np.float32)
    for l in range(NL_ALL):
        for wi, nm in enumerate(("lru_wa", "lru_wx")):
            w = f(nm)[l]
            for c in range(2):
                for gi in range(2):
                    bd[l, 64 * gi:64 * gi + 64, wi * 2 + c, 64 * gi:64 * gi + 64] = w[2 * c + gi]
    c32, cb, kconst, qconst = _prep_consts()
    shared = dict(w_in=np.ascontiguousarray(f("w_in")), w_out=np.ascontiguousarray(f("w_out")),
                  w1=np.ascontiguousarray(f("mlp_w1")), w2=np.ascontiguousarray(f("mlp_w2")),
                  par=par, bd=bd, c32=c32, cb=cb, kconst=kconst, qconst=qconst)
    x = f("x")
    return [dict(shared, x=np.ascontiguousarray(x[b])) for b in range(8)]


_NC_CACHE = {}


def kernel(**inputs):
    in_maps = _prep_inputs(inputs)
    if "nc" not in _NC_CACHE:
        _NC_CACHE["nc"] = build_nc(NL_ALL)
    res = run_bass_kernel_spmd(_NC_CACHE["nc"], in_maps, core_ids=list(range(8)))
    return np.stack([np.asarray(r["out"], dtype=np.float32) for r in res.results], axis=0)
```

```python
import numpy as np
from contextlib import ExitStack
from concourse.bass_utils import run_bass_kernel_spmd
import concourse.bass as bass
import concourse.mybir as mybir

F32 = mybir.dt.float32
BF16 = mybir.dt.bfloat16
AF = mybir.ActivationFunctionType
ALU = mybir.AluOpType
AX = mybir.AxisListType


class Prog:
    ENGS = ("pe", "act", "dve", "pool", "sp")

    def __init__(self, nc):
        self.nc = nc
        self.ops = []
        self.epoch = 0

    def add(self, eng, fn, r=(), w=(), dma=None):
        import sys
        f = sys._getframe(2)
        self.ops.append(dict(eng=eng, fn=fn, r=list(r), w=list(w), dma=dma, epoch=self.epoch, ln=f.f_lineno))

    def pe(self, fn, r=(), w=()):
        self.add("pe", fn, r, w)

    def act(self, fn, r=(), w=()):
        self.add("act", fn, r, w)

    def dve(self, fn, r=(), w=()):
        self.add("dve", fn, r, w)

    def pool(self, fn, r=(), w=()):
        self.add("pool", fn, r, w)

    def dma(self, fn, r=(), w=(), group=None, eng="sp"):
        assert group is not None
        self.add(eng, fn, r, w, dma=group)

    def build(self, stack):
        nc = self.nc
        ops = self.ops
        n = len(ops)
        last_w = {}
        readers = {}
        raw = [set() for _ in range(n)]
        oth = [set() for _ in range(n)]
        for i, op in enumerate(ops):
            rid0 = ("dma", i) if op["dma"] is not None else op["eng"]
            for k in op["r"]:
                if k in last_w:
                    raw[i].add(last_w[k])
                if isinstance(k, tuple) and k[0] == "ps":
                    for rj, j in readers.get(k, {}).items():
                        if rj != rid0:
                            oth[i].add(j)
            for k in op["w"]:
                if k in last_w:
                    oth[i].add(last_w[k])
                for j in readers.get(k, {}).values():
                    oth[i].add(j)
            for k in op["w"]:
                last_w[k] = i
                readers[k] = {}
            rid = ("dma", i) if op["dma"] is not None else op["eng"]
            for k in op["r"]:
                readers.setdefault(k, {})[rid] = i
        need = [[] for _ in range(n)]
        signal = [False] * n
        for i, op in enumerate(ops):
            ds = set()
            for j in raw[i]:
                if j == i:
                    continue
                oj = ops[j]
                if oj["dma"] is None and op["dma"] is None and oj["eng"] == op["eng"] == "pe":
                    continue
                ds.add(j)
            for j in oth[i]:
                if j == i:
                    continue
                oj = ops[j]
                if oj["dma"] is None and op["dma"] is None and oj["eng"] == op["eng"] == "pe":
                    continue
                ds.add(j)
            need[i] = sorted(ds)
            for j in ds:
                signal[j] = True
        for i, op in enumerate(ops):
            if op["dma"] is not None:
                signal[i] = True
        semkeys = {}
        counts = {}
        sigval = [None] * n
        for i, op in enumerate(ops):
            if not signal[i]:
                continue
            key = ("dma", op["dma"]) if op["dma"] is not None else (op["eng"], op["epoch"])
            inc = 16 if op["dma"] is not None else 1
            counts[key] = counts.get(key, 0) + inc
            sigval[i] = (key, counts[key], inc)
        sems = {}
        for key in counts:
            sems[key] = stack.enter_context(nc.semaphore("s_" + "_".join(str(x) for x in key).replace(" ", "")))
        self.nsem = len(sems)
        self.semmap = {str(k): str(v) for k, v in sems.items()}
        import os
        if os.environ.get("DUMPSIG"):
            import json, re
            num = {k: re.search(r"num=(\d+)", str(v)).group(1) for k, v in sems.items()}
            sm = {}
            for i, op in enumerate(ops):
                if sigval[i] is not None:
                    key, val, _ = sigval[i]
                    sm["%s:%d" % (num[key], val)] = [op["ln"], op["eng"], str(key)]
            json.dump(sm, open(os.environ["DUMPSIG"], "w"))
        per_eng = {e: [] for e in self.ENGS}
        for i, op in enumerate(ops):
            per_eng[op["eng"]].append(i)

        def emit_engine(ename, e):
            seen = {}
            for i in per_eng[ename]:
                op = ops[i]
                for j in need[i]:
                    key, val, _ = sigval[j]
                    if seen.get(key, 0) >= val:
                        continue
                    e.wait_ge(sems[key], val)
                    seen[key] = val
                inst = op["fn"](e)
                if signal[i]:
                    key, val, inc = sigval[i]
                    inst.then_inc(sems[key], inc)
            if ename == "sp":
                for key, tot in counts.items():
                    if key[0] == "dma" and seen.get(key, 0) < tot:
                        e.wait_ge(sems[key], tot)

        with nc.Block() as block:
            @block.tensor
            def _(e):
                emit_engine("pe", e)

            @block.scalar
            def _(e):
                emit_engine("act", e)

            @block.vector
            def _(e):
                emit_engine("dve", e)

            @block.gpsimd
            def _(e):
                emit_engine("pool", e)

            @block.sync
            def _(e):
                emit_engine("sp", e)
D = 1024
S = 2048
NL_ALL = 4
NPL = 122
NPAR = NL_ALL * NPL + 64
SPLIT_DMA = False
import os
DBGSKIP = os.environ.get('DBGSKIP', '')
BIG = 32768.0
SLOPES = [2.0 ** (-8.0 * (h + 1) / 4) for h in range(4)]


def build_nc(NL=4, stage=99):
    on = lambda k: 1 if stage >= k else 0
    nc = bass.Bass("TRN2", target_bir_lowering=False)
    dt = nc.dram_tensor
    x_d = dt("x", [S, D], F32, kind="ExternalInput").ap()
    win_d = dt("w_in", [NL_ALL, D, 2560], F32, kind="ExternalInput").ap()
    wout_d = dt("w_out", [NL_ALL, D, D], F32, kind="ExternalInput").ap()
    w1_d = dt("w1", [NL_ALL, D, 4096], F32, kind="ExternalInput").ap()
    w2_d = dt("w2", [NL_ALL, 4096, D], F32, kind="ExternalInput").ap()
    par_d = dt("par", [128, NPAR], F32, kind="ExternalInput").ap()
    bd_d = dt("bd", [NL_ALL, 128, 4, 128], F32, kind="ExternalInput").ap()
    c32_d = dt("c32", [128, 3, 128], F32, kind="ExternalInput").ap()
    cb_d = dt("cb", [128, 2, 128], BF16, kind="ExternalInput").ap()
    kc_d = dt("kconst", [2, 64, S], BF16, kind="ExternalInput").ap()
    qc_d = dt("qconst", [4, 64, S], BF16, kind="ExternalInput").ap()
    out_d = dt("out", [S, D], F32, kind="ExternalOutput").ap()

    st = ExitStack()
    with st:
        P = Prog(nc)
        sb = lambda name, shape, dty: st.enter_context(nc.sbuf_tensor("sb_" + name, shape, dty))
        xT = sb("xT", [128, 8, S], F32)
        hT = sb("hT", [128, 8, 1024], BF16)
        U32 = sb("U", [128, 8192], F32)
        Ub = U32.bitcast(BF16)
        YT = Ub[:, 0:8192].rearrange("p (c t) -> p c t", c=8)
        g0 = Ub[:, 8192:8192 + 2112].rearrange("p (c t) -> p c t", c=2)
        g1 = Ub[:, 10304:10304 + 2048].rearrange("p (c t) -> p c t", c=2)
        dg = Ub[:, 12352:12352 + 31 * 128].rearrange("p (k m) -> p k m", k=31)
        ytmp = U32[:, 4096:8192].rearrange("p (c t) -> p c t", c=8)
        macc = U32[:, :].rearrange("p (c t) -> p c t", c=8)
        kaug = [sb("kaug%d" % h, [128, S], BF16) for h in range(4)]
        qaug = [sb("qaug%d" % h, [128, 1024], BF16) for h in range(4)]
        vaug = sb("vaug", [128, 16, 384], BF16)
        NSTG, NWB = 2, 12
        stg = [sb("stg%d" % i, [128, 1024], F32) for i in range(NSTG)]
        wb = [sb("wb%d" % i, [128, 1024], BF16) for i in range(NWB)]
        par = sb("par", [128, NPAR], F32)
        gmask = par[:, NL_ALL * NPL:NPAR].rearrange("p (a b c) -> p a b c", a=4, b=2)
        c32 = sb("c32", [128, 3, 128], F32)
        cb = sb("cb", [128, 2, 128], BF16)
        onesb = sb("onesb", [128, 128], BF16)
        bdb = sb("bdb", [128, 4, 128], BF16)
        sq4 = sb("sq4", [128, 4, 512], BF16)
        rstd = sb("rstd", [128, 512], F32)
        NWK = 7
        wk = [sb("wk%d" % i, [128, 512], F32) for i in range(NWK)]
        wkb = [w.bitcast(BF16) for w in wk]
        ksum = sb("ksum", [128, 2, 8], F32)
        gt3 = sb("gt3", [128, 3, 8, 8], F32)
        selpad = sb("selpad", [128, 8, 72], BF16)
        lruc = sb("lruc", [128, 8], F32)
        hst = sb("hst", [128, 2], F32)
        tails = sb("tails", [128, 3, 2, 32], BF16)
        dummy = sb("dummy", [128, 4], F32)
        ident32, Sw32, Avg32 = c32[:, 0, :], c32[:, 1, :], c32[:, 2, :]
        identb, trib = cb[:, 0, :], cb[:, 1, :]
        psb = [st.enter_context(nc.psum_tensor("ps%d" % i, [128, 512], F32)) for i in range(8)]

        class Banks:
            def __init__(s, idx, name):
                s.idx, s.i, s.name = idx, 0, name

            def next(s):
                b = s.idx[s.i % len(s.idx)]
                s.i += 1
                return psb[b], ("ps", b)

        bk_mm = Banks([0, 1, 2], "mm")
        bk_sc = Banks([3, 4], "sc")
        bk_pv = Banks([5], "pv")
        bk_st = Banks([6], "st")
        bk_sm = Banks([7], "sm")
        wkc = [0]

        def work():
            i = wkc[0] % NWK
            wkc[0] += 1
            return i

        cnt = dict(stg=0, wb=0)

        def load_slice(src_ap, view3=None):
            slot = cnt["wb"] % NWB
            cnt["wb"] += 1
            o = wb[slot][:]
            if view3:
                o = o.rearrange("p (a b) -> p a b", a=view3)
            P.dma(lambda e: e.dma_start(out=o, in_=src_ap), w=[("wb", slot)], group="wb%d" % slot, eng="pool")
            return wb[slot], ("wb", slot)

        def kslice(w_d, l, c0):
            t, k = load_slice(w_d[l, :, c0:c0 + 128].rearrange("(kc p) c -> p kc c", p=128), view3=8)
            return t[:].rearrange("p (a b) -> p a b", a=8), k

        def pc(l, i):
            return par[:, l * NPL + i:l * NPL + i + 1]

        P.dma(lambda e: e.dma_start(out=par[:], in_=par_d), w=["par"], group="i0")
        P.dma(lambda e: e.dma_start(out=c32[:], in_=c32_d), w=["c32"], group="i1")
        P.dma(lambda e: e.dma_start(out=cb[:], in_=cb_d), w=["cb"], group="i2")
        for h in range(4):
            ty = h % 2
            rows = slice(64, 128) if ty == 0 else slice(0, 64)
            nr = 64
            P.dma(lambda e, h=h, rows=rows, nr=nr, ty=ty: e.dma_start(out=kaug[h][rows, :], in_=kc_d[ty, 0:nr, :]),
                  w=[("kaugc", h)], group="i3_%d" % h)
        P.pool(lambda e: e.memset(onesb[:], 1.0), w=["onesb"])
        P.pool(lambda e: e.memset(vaug[:, :, 64:128], 1.0), w=["vones"])
        P.pool(lambda e: e.memset(vaug[:, :, 256:320], 1.0), w=["vones"])
        P.pool(lambda e: e.memset(selpad[:], 0.0), w=["selpad"])
        P.pool(lambda e: e.memset(dummy[:], 0.0), w=["Uphase"])
        for ts in range(16):
            si = cnt["stg"] % NSTG
            cnt["stg"] += 1
            P.dma(lambda e, si=si, ts=ts: e.dma_start(out=stg[si][:], in_=x_d[ts * 128:(ts + 1) * 128, :]),
                  w=[("stg", si)], group="stg%d" % si)
            for g in range(2):
                pt, pk = bk_sm.next()
                for j in range(4):
                    dc = g * 4 + j
                    P.pe(lambda e, pt=pt, j=j, dc=dc, si=si: e.transpose(pt[:, j * 128:(j + 1) * 128], stg[si][:, dc * 128:(dc + 1) * 128], ident32),
                         r=[("stg", si), "c32"], w=[pk])
                P.act(lambda e, pt=pt, g=g, ts=ts: e.copy(out=xT[:, g * 4:(g + 1) * 4, ts * 128:(ts + 1) * 128],
                                                           in_=pt[:].rearrange("p (a b) -> p a b", a=4)),
                      r=[pk], w=[("xT", dc, ts // 4) for dc in range(g * 4, g * 4 + 4)])

        def barrier():
            P.dve(lambda e: e.memset(dummy[:, 0:1], 0.0), w=["Uphase"])

        UR = ["Uphase"]

        class StatAcc:
            def __init__(s_, pst, pstk, delay=3, slots=None):
                s_.pst, s_.pstk, s_.delay, s_.pend = pst, pstk, delay, []
                s_.slots = slots or [(sq4[:, j, :], ("sq4", j)) for j in range(4)]

            def add(s_, src_ap, rkeys, c):
                sl, slk = s_.slots[c % 4]
                P.act(lambda e, sl=sl: e.activation(out=sl, in_=src_ap, func=AF.Square), r=rkeys, w=[slk])
                s_.pend.append(c)
                while len(s_.pend) > s_.delay:
                    s_.mm(s_.pend.pop(0))

            def mm(s_, c):
                sl, slk = s_.slots[c % 4]
                pst, pstk = s_.pst, s_.pstk
                P.pe(lambda e, sl=sl, c=c, pst=pst: e.matmul(pst[:], lhsT=onesb[:], rhs=sl, start=(c == 0), stop=(c == 7)),
                     r=[slk, "onesb"], w=[pstk])

            def flush(s_):
                while s_.pend:
                    s_.mm(s_.pend.pop(0))

        def rstd_from(pst, pstk, scale, rs=None, rsk="rstd"):
            rs = rstd if rs is None else rs
            P.act(lambda e, pst=pst, rs=rs: e.activation(out=rs[:], in_=pst[:], func=AF.Ln, scale=scale, bias=1e-6), r=[pstk], w=[rsk])
            P.act(lambda e, rs=rs: e.activation(out=rs[:], in_=rs[:], func=AF.Exp, scale=-0.5), r=[rsk], w=[rsk])

        def rms_stats(src4_fn, rkeys_fn, scale):
            pst, pstk = bk_st.next()
            P.act(lambda e: e.activation(out=sq4[:], in_=src4_fn(0), func=AF.Square),
                  r=[k for c in range(0, 4) for k in rkeys_fn(c)], w=[("sq4", j) for j in range(4)])
            dsq = []
            for t2 in range(2):
                wi = work()
                dst = wkb[wi][:, :].rearrange("p (a b) -> p a b", a=2)
                srcv = src4_fn(1)[:, 2 * t2:2 * t2 + 2, :]
                P.dve(lambda e, dst=dst, srcv=srcv: e.tensor_tensor(out=dst, in0=srcv, in1=srcv, op=ALU.mult),
                      r=[k for c in range(4 + 2 * t2, 6 + 2 * t2) for k in rkeys_fn(c)], w=[("wk", wi)])
                dsq.append((dst, ("wk", wi)))
            for c in range(8):
                if c < 4:
                    rhs, rk = sq4[:, c, :], ("sq4", c)
                else:
                    dst, rk = dsq[(c - 4) // 2]
                    rhs = dst[:, (c - 4) % 2, :]
                P.pe(lambda e, c=c, pst=pst, rhs=rhs: e.matmul(pst[:], lhsT=onesb[:], rhs=rhs, start=(c == 0), stop=(c == 7)),
                     r=[rk, "onesb"], w=[pstk])
            rstd_from(pst, pstk, scale)
            return
            for g in range(2):
                P.act(lambda e, g=g: e.activation(out=sq4[:], in_=src4_fn(g), func=AF.Square),
                      r=[k for c in range(4 * g, 4 * g + 4) for k in rkeys_fn(c)], w=[("sq4", j) for j in range(4)])
                for j in range(4):
                    c = 4 * g + j
                    P.pe(lambda e, j=j, c=c, pst=pst: e.matmul(pst[:], lhsT=onesb[:], rhs=sq4[:, j, :], start=(c == 0), stop=(c == 7)),
                         r=[("sq4", j), "onesb"], w=[pstk])
            rstd_from(pst, pstk, scale)

        def apply_h(l, tt, gbase, rs=None, rsk="rstd"):
            lt = tt % 2
            rs = rstd if rs is None else rs
            for c in range(8):
                P.dve(lambda e, c=c, rs=rs: e.scalar_tensor_tensor(out=hT[:, c, lt * 512:(lt + 1) * 512], in0=xT[:, c, tt * 512:(tt + 1) * 512],
                                                                   scalar=pc(l, gbase + c), in1=rs[:], op0=ALU.mult, op1=ALU.mult),
                      r=[("xT", c, tt), rsk, "par"], w=[("hT", c, lt)])

        def update_x(l, tt, src, srckey_fn, gbase, next_stats=None):
            for dc in range(8):
                update_chunk(l, tt, src, srckey_fn, gbase, dc, next_stats)
            if next_stats is not None:
                next_stats.flush()

        def update_chunk(l, tt, src, srckey_fn, gbase, dc, next_stats=None):
            P.dve(lambda e, dc=dc: e.tensor_tensor(out=src(dc), in0=src(dc), in1=rstd[:], op=ALU.mult), r=[srckey_fn(dc), "rstd"] + UR, w=[srckey_fn(dc)])
            P.dve(lambda e, dc=dc: e.scalar_tensor_tensor(out=xT[:, dc, tt * 512:(tt + 1) * 512], in0=src(dc), scalar=pc(l, gbase + dc),
                                                          in1=xT[:, dc, tt * 512:(tt + 1) * 512], op0=ALU.mult, op1=ALU.add),
                  r=[srckey_fn(dc), ("xT", dc, tt), "par"] + UR, w=[("xT", dc, tt)])
            if next_stats is not None:
                next_stats.add(xT[:, dc, tt * 512:(tt + 1) * 512], [("xT", dc, tt)], dc)

        def norm_to_h(l, tt, gbase):
            rms_stats(lambda g: xT[:, 4 * g:4 * g + 4, tt * 512:(tt + 1) * 512], lambda c: [("xT", c, tt)], 1.0 / D)
            apply_h(l, tt, gbase)

        def proj(wt, wkey, tt):
            lt = tt % 2
            pt, pk = bk_mm.next()
            for kc in range(8):
                P.pe(lambda e, kc=kc, pt=pt: e.matmul(pt[:], lhsT=wt[:, kc, :], rhs=hT[:, kc, lt * 512:(lt + 1) * 512], start=(kc == 0), stop=(kc == 7)),
                     r=[wkey, ("hT", kc, lt)], w=[pk])
            return pt, pk

        def build_dg(l, base, ntap, c):
            for k in range(ntap):
                P.dve(lambda e, k=k: e.tensor_scalar(out=dg[:, k, :], in0=identb, scalar1=pc(l, base + k * 2 + c), scalar2=None, op0=ALU.mult),
                      r=["cb", "par"] + UR, w=[("dg", k)])

        def conv(ntap, c, tt, grp):
            lt = tt % 2
            pt, pk = bk_mm.next()
            for k in range(ntap):
                off = 32 - (ntap - 1) + k + lt * 512
                P.pe(lambda e, k=k, off=off, pt=pt: e.matmul(pt[:], lhsT=dg[:, k, :], rhs=g0[:, c, off:off + 512], start=(k == 0), stop=(k == ntap - 1)),
                     r=[("dg", k), ("g0", c, lt), ("g0", c, lt - 1)] + UR, w=[pk])
            return pt, pk

        def pad_in(hf, c, grp):
            if hf == 0:
                P.dve(lambda e: e.memset(g0[:, c, 0:32], 0.0), r=UR, w=[("g0", c, -1)])
            else:
                P.dve(lambda e: e.tensor_copy(out=g0[:, c, 0:32], in_=tails[:, grp, c, :]), r=[("tails", grp, c)] + UR, w=[("g0", c, -1)])

        def pad_out(hf, c, grp):
            if hf == 0:
                P.dve(lambda e: e.tensor_copy(out=tails[:, grp, c, :], in_=g0[:, c, 1024:1056]), r=[("g0", c, 1)] + UR, w=[("tails", grp, c)])

        for l in range(NL):
            P.epoch = l
            if on(0.1):
                P.act(lambda e, l=l: e.activation(out=lruc[:, 0:2], in_=par[:, l * NPL + 44:l * NPL + 46], func=AF.Exp, scale=-1.0), r=["par"], w=["lruc0"])
                P.act(lambda e: e.activation(out=lruc[:, 2:4], in_=lruc[:, 0:2], func=AF.Ln, bias=1.0), r=["lruc0"], w=["lruc1"])
                P.dve(lambda e: e.tensor_scalar(out=lruc[:, 4:6], in0=lruc[:, 2:4], scalar1=-8.0, scalar2=None, op0=ALU.mult), r=["lruc1"], w=["lruc2"])
                P.dve(lambda e: e.tensor_scalar(out=lruc[:, 6:8], in0=lruc[:, 2:4], scalar1=-16.0, scalar2=None, op0=ALU.mult), r=["lruc1"], w=["lruc3"])
                si = cnt["stg"] % NSTG
                cnt["stg"] += 1
                P.dma(lambda e, si=si, l=l: e.dma_start(out=stg[si][:, 0:512].rearrange("p (a b) -> p a b", a=4), in_=bd_d[l]), w=[("stg", si)], group="stg%d" % si)
                P.dve(lambda e, si=si: e.tensor_copy(out=bdb[:].rearrange("p a b -> p (a b)"), in_=stg[si][:, 0:512]), r=[("stg", si)], w=["bdb"])

            for hf in range(2):
                tts = [2 * hf, 2 * hf + 1]
                barrier()
                for h in range(4 * on(0.5)):
                    ty = h % 2
                    rows = slice(64, 128) if ty == 0 else slice(0, 64)
                    nr = 64
                    P.dma(lambda e, h=h, rows=rows, nr=nr, hf=hf: e.dma_start(out=qaug[h][rows, :], in_=qc_d[h, 0:nr, hf * 1024:(hf + 1) * 1024]),
                          w=[("qaugc", h)], group="qc%d" % h)
                kws = [kslice(win_d, l, 1536 + c2 * 128) for c2 in range(2)]
                for tt in tts:
                    norm_to_h(l, tt, 0)
                    for c2 in range(2):
                        wt, wkey = kws[c2]
                        pt, pk = proj(wt, wkey, tt)
                        P.act(lambda e, pt=pt, c2=c2, tt=tt: e.copy(out=kaug[2 * c2][0:64, tt * 512:(tt + 1) * 512], in_=pt[0:64, :]), r=[pk], w=[("kaug", 2 * c2, tt)])
                        P.act(lambda e, pt=pt, c2=c2, tt=tt: e.copy(out=kaug[2 * c2 + 1][64:128, tt * 512:(tt + 1) * 512], in_=pt[64:128, :]), r=[pk], w=[("kaug", 2 * c2 + 1, tt)])
                        P.dve(lambda e, pt=pt, c2=c2, tt=tt: e.tensor_reduce(out=ksum[:, c2, 2 * tt:2 * tt + 2], in_=pt[:].rearrange("p (a b) -> p a b", a=2), axis=AX.X, op=ALU.add),
                              r=[pk], w=[("ksum", c2, tt)])
                vs = [kslice(win_d, l, 1792 + j * 128) for j in range(2 * on(2))]
                for ts in range(8 * hf, 8 * hf + 8 * on(2)):
                    lts = ts - 8 * hf
                    pt, pk = bk_mm.next()
                    for j in range(2):
                        wt, wkey = vs[j]
                        for kc in range(8):
                            P.pe(lambda e, pt=pt, j=j, kc=kc, wt=wt, lts=lts: e.matmul(pt[:, j * 128:(j + 1) * 128], lhsT=hT[:, kc, lts * 128:(lts + 1) * 128], rhs=wt[:, kc, :],
                                                                                       start=(kc == 0), stop=(kc == 7)),
                                 r=[wkey, ("hT", kc, lts // 4)], w=[pk])
                    vv = vaug[:, ts, :].rearrange("p (a b c) -> p a b c", a=2, b=3)
                    for pr in range(2):
                        for od in range(2):
                            P.act(lambda e, pt=pt, pr=pr, od=od, vv=vv: e.copy(out=vv[:, pr, 2 * od, :], in_=pt[:, (2 * pr + od) * 64:(2 * pr + od + 1) * 64]),
                                  r=[pk], w=[("vaug", ts)])
                bw = {}

                def mk_bstep(c, tt, l=l, hf=hf):
                    def step():
                        if c not in bw:
                            pad_in(hf, c, 1)
                            bw[c] = (kslice(win_d, l, 1024 + c * 128), kslice(win_d, l, 768 + c * 128), kslice(win_d, l, 512 + c * 128))
                        (wx_, wxk), (wc_, wck), (wb_, wbk) = bw[c]
                        lt = tt % 2
                        px_, pxk_ = proj(wx_, wxk, tt)
                        xi = work()
                        xs32 = wk[xi]
                        P.dve(lambda e, px_=px_, xs32=xs32: e.tensor_copy(out=xs32[:], in_=px_[:]), r=[pxk_], w=[("wk", xi)])
                        pc_, pck_ = proj(wc_, wck, tt)
                        P.dve(lambda e, pc_=pc_, xs32=xs32, c=c, lt=lt: e.tensor_tensor(out=g0[:, c, 32 + lt * 512:32 + (lt + 1) * 512], in0=pc_[:], in1=xs32[:], op=ALU.mult),
                              r=[pck_, ("wk", xi)] + UR, w=[("g0", c, lt)])
                        pb_, pbk_ = proj(wb_, wbk, tt)
                        P.dve(lambda e, pb_=pb_, c=c, lt=lt: e.tensor_copy(out=g1[:, c, lt * 512:(lt + 1) * 512], in_=pb_[:]), r=[pbk_] + UR, w=[("g1", c, lt)])
                    return step

                bsteps = [mk_bstep(c, tt) for c in range(2 * on(6)) for tt in tts]
                qs = [kslice(win_d, l, 1280 + j * 128) for j in range(2 * on(3))]
                for tt in tts[:2 * on(3)]:
                    lt = tt % 2
                    for c2 in range(2):
                        wt, wkey = qs[c2]
                        pt, pk = proj(wt, wkey, tt)
                        hA, hB = 2 * c2, 2 * c2 + 1
                        P.act(lambda e, pt=pt, hA=hA, lt=lt: e.activation(out=qaug[hA][0:64, lt * 512:(lt + 1) * 512], in_=pt[0:64, :], func=AF.Identity, scale=0.125, bias=0.0),
                              r=[pk], w=[("qaug", hA, lt)])
                        P.act(lambda e, pt=pt, hB=hB, lt=lt: e.activation(out=qaug[hB][64:128, lt * 512:(lt + 1) * 512], in_=pt[64:128, :], func=AF.Identity, scale=0.125, bias=0.0),
                              r=[pk], w=[("qaug", hB, lt)])
                        if tt >= 2:
                            qi = work()
                            q32 = wk[qi]
                            P.act(lambda e, pt=pt, q32=q32: e.copy(out=q32[:], in_=pt[:]), r=[pk], w=[("wk", qi)])
                            gps, gpsk = bk_sm.next()
                            for sub in range(4):
                                for ty in range(2):
                                    i8 = sub * 2 + ty
                                    rows = slice(0, 64) if ty == 0 else slice(64, 128)
                                    P.pe(lambda e, gps=gps, q32=q32, rows=rows, sub=sub, c2=c2, i8=i8: e.matmul(gps[:, i8 * 8:(i8 + 1) * 8], lhsT=q32[rows, sub * 128:(sub + 1) * 128], rhs=ksum[rows, c2, :], start=True, stop=True),
                                         r=[("wk", qi)] + [("ksum", c2, t2) for t2 in range(4)], w=[gpsk])
                            tps = [(psb[6], ("ps", 6)), (psb[5], ("ps", 5))]
                            for sub in range(4):
                                own = (tt * 512 + sub * 128) // 256
                                for ty in range(2):
                                    i8 = sub * 2 + ty
                                    off = 64 if ty == 0 else 32
                                    gk = ("gt", i8)
                                    P.dve(lambda e, gps=gps, i8=i8, own=own: e.tensor_tensor(out=gt3[:, 0, i8, :], in0=gps[:, i8 * 8:(i8 + 1) * 8], in1=gmask[:, own - 4, 0, :], op=ALU.add),
                                          r=[gpsk, "par"], w=[gk])
                                    P.dve(lambda e, gps=gps, i8=i8, own=own: e.tensor_tensor(out=gt3[:, 1, i8, :], in0=gps[:, i8 * 8:(i8 + 1) * 8], in1=gmask[:, own - 4, 1, :], op=ALU.add),
                                          r=[gpsk, "par"], w=[gk])
                                    P.dve(lambda e, i8=i8: e.max(out=gt3[:, 2, i8, :], in_=gt3[:, 0, i8, :]), r=[gk], w=[gk])
                                    P.dve(lambda e, i8=i8, off=off: e.tensor_scalar(out=selpad[:, i8, off:off + 8], in0=gt3[:, 1, i8, :], scalar1=gt3[:, 2, i8, 2:3], scalar2=-BIG, op0=ALU.is_lt, op1=ALU.mult),
                                          r=[gk], w=[("selpad", i8)])
                                    tp, tpk = tps[ty]
                                    P.pe(lambda e, tp=tp, off=off, i8=i8, sub=sub: e.matmul(tp[0:off + 8, sub * 128:(sub + 1) * 128], lhsT=selpad[:, i8, 0:off + 8], rhs=identb, start=True, stop=True),
                                         r=[("selpad", i8), "cb"], w=[tpk])
                            for ty in range(2):
                                h = 2 * c2 + ty
                                off = 64 if ty == 0 else 32
                                tp, tpk = tps[ty]
                                P.act(lambda e, tp=tp, off=off, h=h, lt=lt: e.copy(out=qaug[h][off:off + 8, lt * 512:(lt + 1) * 512], in_=tp[off:off + 8, :]),
                                      r=[tpk, ("qaugc", h)], w=[("qaug", h, lt)])
                    for h in range(4 * on(4)):
                        ty = h % 2
                        rows = slice(0, 128)
                        nrows = slice(0, 64) if ty == 0 else slice(64, 128)
                        pr = h // 2
                        vc0 = pr * 192 + (0 if ty == 0 else 64)
                        nkt = (tt + 1) * 4
                        pv, pvk = bk_pv.next()
                        def emit_sc(kt, h=h, tt=tt, lt=lt):
                            o = kt - tt * 4
                            n0 = max(o, 0) * 128
                            sc, sck = bk_sc.next()
                            P.pe(lambda e, sc=sc, h=h, kt=kt, n0=n0, o=o, lt=lt: e.matmul(sc[:, n0:512], lhsT=kaug[h][:, kt * 128:(kt + 1) * 128],
                                                                                     rhs=qaug[h][:, lt * 512 + n0:(lt + 1) * 512], start=True, stop=(o < 0)),
                                 r=[("kaug", h, kt // 4), ("kaugc", h), ("qaug", h, lt), ("qaugc", h)], w=[sck])
                            if o >= 0:
                                P.pe(lambda e, sc=sc, n0=n0: e.matmul(sc[:, n0:n0 + 128], lhsT=identb, rhs=trib, start=False, stop=True), r=["cb"], w=[sck])
                            pi = work()
                            PT = wkb[pi][:, 0:512]
                            P.act(lambda e, sc=sc, PT=PT, n0=n0: e.activation(out=PT[:, n0:512], in_=sc[:, n0:512], func=AF.Exp), r=[sck], w=[("wk", pi)])
                            return PT, pi, n0

                        def emit_pv(kt, info, pv=pv, pvk=pvk, vc0=vc0, nkt=nkt):
                            PT, pi, n0 = info
                            P.pe(lambda e, pv=pv, PT=PT, n0=n0, kt=kt, vc0=vc0, nkt=nkt: e.matmul(pv[:, n0:512], lhsT=vaug[:, kt, vc0:vc0 + 128], rhs=PT[:, n0:512],
                                                                                                start=(kt == 0), stop=(kt == nkt - 1)),
                                 r=[("wk", pi), ("vaug", kt), "vones"], w=[pvk])

                        infos = {0: emit_sc(0)}
                        for kt in range(nkt):
                            if kt + 1 < nkt:
                                infos[kt + 1] = emit_sc(kt + 1)
                            emit_pv(kt, infos.pop(kt))
                        si_ = work()
                        pvs = wk[si_]
                        P.dve(lambda e, pv=pv, pvs=pvs: e.tensor_copy(out=pvs[:], in_=pv[:]), r=[pvk], w=[("wk", si_)])
                        pst, pstk = bk_st.next()
                        P.pe(lambda e, pst=pst, pvs=pvs: e.matmul(pst[:], lhsT=Sw32, rhs=pvs[:], start=True, stop=True), r=[("wk", si_), "c32"], w=[pstk])
                        ri = work()
                        rec = wk[ri]
                        P.act(lambda e, pst=pst, rec=rec, nrows=nrows: e.activation(out=rec[nrows, :], in_=pst[nrows, :], func=AF.Ln), r=[pstk], w=[("wk", ri)])
                        P.act(lambda e, rec=rec, nrows=nrows: e.activation(out=rec[nrows, :], in_=rec[nrows, :], func=AF.Exp, scale=-1.0), r=[("wk", ri)], w=[("wk", ri)])
                        P.dve(lambda e, pvs=pvs, rec=rec, nrows=nrows, pr=pr, lt=lt: e.tensor_tensor(out=YT[nrows, 4 + pr, lt * 512:(lt + 1) * 512], in0=pvs[nrows, :], in1=rec[nrows, :], op=ALU.mult),
                              r=[("wk", si_), ("wk", ri)] + UR, w=[("YT", 4 + pr, lt, ty)])
                        if h % 2 == 1 and bsteps:
                            bsteps.pop(0)()

                while bsteps:
                    bsteps.pop(0)()
                for c in range(2 * on(6)):
                    build_dg(l, 108, 3, c)
                    pad_out(hf, c, 1)
                    for tt in tts:
                        lt = tt % 2
                        pt, pk = conv(3, c, tt, 1)
                        P.dve(lambda e, pt=pt, c=c, lt=lt: e.tensor_tensor(out=YT[:, 2 + c, lt * 512:(lt + 1) * 512], in0=pt[:], in1=g1[:, c, lt * 512:(lt + 1) * 512], op=ALU.mult),
                              r=[pk, ("g1", c, lt)] + UR, w=[("YT", 2 + c, lt)])

                for c in range(2 * on(5)):
                    build_dg(l, 46, 31, c)
                    pad_in(hf, c, 0)
                    wv_, wvk = kslice(win_d, l, c * 128)
                    wg_, wgk = kslice(win_d, l, 256 + c * 128)
                    for tt in tts:
                        lt = tt % 2
                        pv_, pvk_ = proj(wv_, wvk, tt)
                        pg_, pgk_ = proj(wg_, wgk, tt)
                        si_ = work()
                        sg = wk[si_]
                        P.act(lambda e, pg_=pg_, sg=sg: e.activation(out=sg[:], in_=pg_[:], func=AF.Sigmoid), r=[pgk_], w=[("wk", si_)])
                        P.dve(lambda e, pv_=pv_, sg=sg, c=c, lt=lt: e.tensor_tensor(out=g0[:, c, 32 + lt * 512:32 + (lt + 1) * 512], in0=pv_[:], in1=sg[:], op=ALU.mult),
                              r=[pvk_, ("wk", si_)] + UR, w=[("g0", c, lt)])
                    pad_out(hf, c, 0)
                    bk_gn = Banks([3, 4, 5, 6], "gn")
                    ZC = {}
                    for tt in tts:
                        pt, pk = conv(31, c, tt, 0)
                        zi = work()
                        zc = wk[zi]
                        P.act(lambda e, l=l, pt=pt, zc=zc, c=c: e.activation(out=zc[:], in_=pt[:], func=AF.Identity, bias=pc(l, 32 + c), scale=1.0), r=[pk, "par"], w=[("wk", zi)])
                        ZC[tt] = (zc, zi)
                    SQ = {}
                    for tt in tts:
                        zc, zi = ZC[tt]
                        pst, pstk = bk_gn.next()
                        P.pe(lambda e, pst=pst, zc=zc: e.matmul(pst[:], lhsT=Avg32, rhs=zc[:], start=True, stop=True), r=[("wk", zi), "c32"], w=[pstk])
                        P.dve(lambda e, pst=pst, zc=zc: e.tensor_tensor(out=zc[:], in0=zc[:], in1=pst[:], op=ALU.subtract), r=[("wk", zi), pstk], w=[("wk", zi)])
                        qi = work()
                        s32 = wk[qi]
                        P.act(lambda e, zc=zc, s32=s32: e.activation(out=s32[:], in_=zc[:], func=AF.Square), r=[("wk", zi)], w=[("wk", qi)])
                        SQ[tt] = (s32, qi)
                    for tt in tts:
                        lt = tt % 2
                        zc, zi = ZC[tt]
                        s32, qi = SQ[tt]
                        pst2, pstk2 = bk_gn.next()
                        P.pe(lambda e, pst2=pst2, s32=s32: e.matmul(pst2[:], lhsT=Avg32, rhs=s32[:], start=True, stop=True), r=[("wk", qi), "c32"], w=[pstk2])
                        P.act(lambda e, pst2=pst2, s32=s32: e.activation(out=s32[:], in_=pst2[:], func=AF.Ln, bias=1e-5, scale=1.0), r=[pstk2], w=[("wk", qi)])
                        P.act(lambda e, s32=s32: e.activation(out=s32[:], in_=s32[:], func=AF.Exp, scale=-0.5), r=[("wk", qi)], w=[("wk", qi)])
                        P.dve(lambda e, zc=zc, s32=s32: e.tensor_tensor(out=zc[:], in0=zc[:], in1=s32[:], op=ALU.mult), r=[("wk", zi), ("wk", qi)], w=[("wk", zi)])
                        P.act(lambda e, l=l, zc=zc, c=c, lt=lt: e.activation(out=YT[:, c, lt * 512:(lt + 1) * 512], in_=zc[:], func=AF.Silu, scale=pc(l, 34 + c), bias=pc(l, 36 + c)),
                              r=[("wk", zi), "par"] + UR, w=[("YT", c, lt)])

                for c in range(2 * on(7)):
                    build_dg(l, 114, 4, c)
                    pad_in(hf, c, 2)
                    wr_, wrk = kslice(win_d, l, 2048 + c * 128)
                    wg_, wgk = kslice(win_d, l, 2304 + c * 128)
                    for tt in tts:
                        lt = tt % 2
                        pr_, prk_ = proj(wr_, wrk, tt)
                        P.act(lambda e, pr_=pr_, c=c, lt=lt: e.copy(out=g0[:, c, 32 + lt * 512:32 + (lt + 1) * 512], in_=pr_[:]), r=[prk_] + UR, w=[("g0", c, lt)])
                        pg_, pgk_ = proj(wg_, wgk, tt)
                        P.act(lambda e, pg_=pg_, c=c, lt=lt: e.activation(out=g1[:, c, lt * 512:(lt + 1) * 512], in_=pg_[:], func=AF.Gelu_apprx_tanh), r=[pgk_] + UR, w=[("g1", c, lt)])
                    pad_out(hf, c, 2)
                    bk_bd = Banks([3, 4, 5, 6], "bd")
                    XC = {}
                    for n_, tt in enumerate(tts):
                        pt, pk = conv(4, c, tt, 2)
                        xi = n_
                        xc32 = wk[xi]
                        xcb = wkb[2][:, n_ * 512:(n_ + 1) * 512]
                        P.act(lambda e, l=l, pt=pt, xc32=xc32, c=c: e.activation(out=xc32[:], in_=pt[:], func=AF.Identity, bias=pc(l, 38 + c), scale=1.0), r=[pk, "par"], w=[("wk", xi)])
                        P.act(lambda e, l=l, pt=pt, xcb=xcb, c=c: e.activation(out=xcb, in_=pt[:], func=AF.Identity, bias=pc(l, 38 + c), scale=1.0), r=[pk, "par"], w=[("wk", 2)])
                        XC[tt] = (xc32, xi, xcb)
                    PA = {}
                    for tt in tts:
                        xc32, xi, xcb = XC[tt]
                        pa, pak = bk_bd.next()
                        P.pe(lambda e, pa=pa, xcb=xcb, c=c: e.matmul(pa[:], lhsT=bdb[:, c, :], rhs=xcb, start=True, stop=True), r=[("wk", 2), "bdb"], w=[pak])
                        px, pxk = bk_bd.next()
                        P.pe(lambda e, px=px, xcb=xcb, c=c: e.matmul(px[:], lhsT=bdb[:, 2 + c, :], rhs=xcb, start=True, stop=True), r=[("wk", 2), "bdb"], w=[pxk])
                        PA[tt] = (pa, pak, px, pxk)
                    for n_, tt in enumerate(tts):
                        lt = tt % 2
                        xc32, xi, xcb = XC[tt]
                        pa, pak, px, pxk = PA[tt]
                        r_i, i_i, e_i = (3, 4, 5) if n_ == 0 else (6, 3, 4)
                        rt, it, et = wk[r_i], wk[i_i], wk[e_i]
                        P.act(lambda e, l=l, pa=pa, rt=rt, c=c: e.activation(out=rt[:], in_=pa[:], func=AF.Sigmoid, bias=pc(l, 40 + c), scale=1.0), r=[pak, "par"], w=[("wk", r_i)])
                        P.act(lambda e, l=l, px=px, it=it, c=c: e.activation(out=it[:], in_=px[:], func=AF.Sigmoid, bias=pc(l, 42 + c), scale=1.0), r=[pxk, "par"], w=[("wk", i_i)])
                        P.act(lambda e, rt=rt, et=et, c=c: e.activation(out=et[:], in_=rt[:], func=AF.Exp, scale=lruc[:, 6 + c:7 + c]), r=[("wk", r_i), "lruc3"], w=[("wk", e_i)])
                        P.act(lambda e, rt=rt, c=c: e.activation(out=rt[:], in_=rt[:], func=AF.Exp, scale=lruc[:, 4 + c:5 + c]), r=[("wk", r_i), "lruc2", ("wk", e_i)], w=[("wk", r_i)])
                        P.act(lambda e, et=et: e.activation(out=et[:], in_=et[:], func=AF.Sqrt, scale=-1.0, bias=1.0), r=[("wk", e_i)], w=[("wk", e_i)])
                        P.dve(lambda e, it=it, xc32=xc32: e.tensor_tensor(out=it[:], in0=it[:], in1=xc32[:], op=ALU.mult), r=[("wk", i_i), ("wk", xi)], w=[("wk", i_i)])
                        P.dve(lambda e, it=it, et=et: e.tensor_tensor(out=it[:], in0=it[:], in1=et[:], op=ALU.mult), r=[("wk", i_i), ("wk", e_i)], w=[("wk", i_i)])
                        init = 0.0 if tt == 0 else hst[:, c:c + 1]
                        P.dve(lambda e, et=et, rt=rt, it=it, init=init: e.tensor_tensor_scan(out=et[:], data0=rt[:], data1=it[:], initial=init, op0=ALU.mult, op1=ALU.add),
                              r=[("wk", r_i), ("wk", i_i), ("hst", c)], w=[("wk", e_i)])
                        P.dve(lambda e, et=et, c=c: e.tensor_copy(out=hst[:, c:c + 1], in_=et[:, 511:512]), r=[("wk", e_i)], w=[("hst", c)])
                        P.dve(lambda e, et=et, c=c, lt=lt: e.tensor_tensor(out=YT[:, 6 + c, lt * 512:(lt + 1) * 512], in0=et[:], in1=g1[:, c, lt * 512:(lt + 1) * 512], op=ALU.mult),
                              r=[("wk", e_i), ("g1", c, lt)] + UR, w=[("YT", 6 + c, lt)])

                barrier()
                wos = [kslice(wout_d, l, dc * 128) for dc in range(8 * on(8))]
                ytk = lambda c: [("YT", c, lt)] if c not in (4, 5) else [("YT", c, lt, 0), ("YT", c, lt, 1)]
                if on(8):
                    pstA, pstAk = psb[6], ("ps", 6)
                    pstB, pstBk = psb[7], ("ps", 7)
                    sbt = [work(), work()]
                    slotsB = [(wkb[sbt[j // 2]][:, (j % 2) * 512:(j % 2 + 1) * 512], ("wk", sbt[j // 2])) for j in range(4)]
                    rs2i = work()
                    rs2, rs2k = wk[rs2i], ("wk", rs2i)
                    ysrc = lambda dc: ytmp[:, dc, :]
                    ykey = lambda dc: ("ytmp", dc)

                    def wout_group(tt, dc, stA):
                        lt = tt % 2
                        wt, wkey = wos[dc]
                        pt, pk = bk_mm.next()
                        ytk_ = lambda c: [("YT", c, lt)] if c not in (4, 5) else [("YT", c, lt, 0), ("YT", c, lt, 1)]
                        for c in range(8):
                            P.pe(lambda e, pt=pt, wt=wt, c=c, lt=lt: e.matmul(pt[:], lhsT=wt[:, c, :], rhs=YT[:, c, lt * 512:(lt + 1) * 512], start=(c == 0), stop=(c == 7)),
                                 r=[wkey] + ytk_(c) + UR, w=[pk])
                        P.act(lambda e, pt=pt, dc=dc: e.copy(out=ytmp[:, dc, :], in_=pt[:]), r=[pk] + UR, w=[("ytmp", dc)])
                        stA.add(pt[:], [pk], dc)

                    tt0, tt1 = tts
                    stA0 = StatAcc(pstA, pstAk)
                    for dc in range(8):
                        wout_group(tt0, dc, stA0)
                    stA0.flush()
                    rstd_from(pstA, pstAk, 1.0 / D)
                    stB0 = StatAcc(pstB, pstBk, slots=slotsB)
                    stA1 = StatAcc(pstA, pstAk)
                    for dc in range(8):
                        update_chunk(l, tt0, ysrc, ykey, 8, dc, stB0)
                        wout_group(tt1, dc, stA1)
                    stB0.flush()
                    stA1.flush()
                    rstd_from(pstB, pstBk, 1.0 / D, rs2, rs2k)
                    apply_h(l, tt0, 16, rs2, rs2k)
                    rstd_from(pstA, pstAk, 1.0 / D)
                    stB1 = StatAcc(pstB, pstBk, slots=slotsB)
                    for dc in range(8):
                        update_chunk(l, tt1, ysrc, ykey, 8, dc, stB1)
                    stB1.flush()
                    rstd_from(pstB, pstBk, 1.0 / D, rs2, rs2k)
                    apply_h(l, tt1, 16, rs2, rs2k)

                barrier()
                bk_y = Banks([3, 4, 5], "y")
                w1c, w2c = {}, {}
                mstat = {}

                def W1(fg, l=l):
                    if fg not in w1c:
                        w1c[fg] = [kslice(w1_d, l, fg * 512 + j * 128) for j in range(4)]
                    return w1c[fg]

                def W2(fg, l=l):
                    if fg not in w2c:
                        w2c[fg] = [load_slice(w2_d[l, fg * 512 + j * 128:fg * 512 + (j + 1) * 128, :]) for j in range(4)]
                    return w2c[fg]

                def emit_hid(fg, tt):
                    hid = []
                    hi = None
                    for j in range(4):
                        wt, wkey = W1(fg)[j]
                        pt, pk = proj(wt, wkey, tt)
                        if j % 2 == 0:
                            hi = work()
                        hd = wkb[hi][:, (j % 2) * 512:(j % 2 + 1) * 512]
                        hk = ("wk", hi)
                        P.act(lambda e, pt=pt, hd=hd: e.activation(out=hd, in_=pt[:], func=AF.Relu), r=[pk], w=[hk])
                        P.dve(lambda e, hd=hd, pt=pt: e.tensor_tensor(out=hd, in0=hd, in1=pt[:], op=ALU.mult), r=[hk, pk], w=[hk])
                        hid.append((hd, hk, hi))
                    return hid

                def emit_y(fg, tt, hid, hook=None):
                    lt = tt % 2
                    for dc in range(8):
                        pt, pk = bk_y.next()
                        for j in range(4):
                            wt, wkey = W2(fg)[j]
                            hd, hk, hi = hid[j]
                            P.pe(lambda e, pt=pt, wt=wt, hd=hd, dc=dc, j=j: e.matmul(pt[:], lhsT=wt[:, dc * 128:(dc + 1) * 128], rhs=hd, start=(j == 0), stop=(j == 3)),
                                 r=[wkey, hk], w=[pk])
                        mo = macc[:, dc, lt * 512:(lt + 1) * 512]
                        if fg == 0:
                            P.act(lambda e, pt=pt, mo=mo: e.copy(out=mo, in_=pt[:]), r=[pk] + UR, w=[("macc", dc, lt)])
                        else:
                            P.dve(lambda e, pt=pt, mo=mo: e.tensor_tensor(out=mo, in0=mo, in1=pt[:], op=ALU.add), r=[pk, ("macc", dc, lt)] + UR, w=[("macc", dc, lt)])
                        if fg == 7:
                            if dc == 0:
                                mstat[lt] = StatAcc(psb[6 + lt], ("ps", 6 + lt))
                            mstat[lt].add(mo, [("macc", dc, lt)] + UR, dc)
                            if dc == 7:
                                mstat[lt].flush()
                        if hook is not None:
                            hook(dc)

                its = [(fg, tt) for fg in range(8 * on(9)) for tt in tts]
                hids = {}
                if its:
                    hids[0] = emit_hid(*its[0])
                for n, (fg, tt) in enumerate(its):
                    if n + 1 < len(its):
                        hids[n + 1] = emit_hid(*its[n + 1])
                    hook = None
                    if fg == 7 and tt == tts[1]:
                        rstd_from(psb[6], ("ps", 6), 1.0 / D)
                        hook = lambda dc, l=l, t0=tts[0]: update_chunk(l, t0, lambda d_: macc[:, d_, 0:512], lambda d_: ("macc", d_, 0), 24, dc)
                    emit_y(fg, tt, hids.pop(n), hook)
                for tt in tts[1:2 * on(9)]:
                    lt = tt % 2
                    rstd_from(psb[6 + lt], ("ps", 6 + lt), 1.0 / D)
                    update_x(l, tt, lambda dc, lt=lt: macc[:, dc, lt * 512:(lt + 1) * 512], lambda dc, lt=lt: ("macc", dc, lt), 24)

        for ts in range(16):
            si = cnt["stg"] % NSTG
            cnt["stg"] += 1
            for g in range(2):
                pt, pk = bk_sm.next()
                for j in range(4):
                    dc = g * 4 + j
                    P.pe(lambda e, pt=pt, j=j, dc=dc, ts=ts: e.transpose(pt[:, j * 128:(j + 1) * 128], xT[:, dc, ts * 128:(ts + 1) * 128], ident32),
                         r=[("xT", dc, ts // 4), "c32"], w=[pk])
                P.act(lambda e, pt=pt, g=g, si=si: e.copy(out=stg[si][:, g * 512:(g + 1) * 512], in_=pt[:]), r=[pk], w=[("stg", si)])
            P.dma(lambda e, si=si, ts=ts: e.dma_start(out=out_d[ts * 128:(ts + 1) * 128, :], in_=stg[si][:]), r=[("stg", si)], group="stg%d" % si)
        P.build(st)
        print("ops", len(P.ops), "sems", P.nsem)
    return nc


def _prep_consts():
    import ml_dtypes
    bf = ml_dtypes.bfloat16
    ident = np.eye(128, dtype=np.float32)
    sw = np.zeros((128, 128), np.float32)
    for i in range(128):
        sw[i, (i + 64) % 128] = 1.0
    avg = np.zeros((128, 128), np.float32)
    avg[0:64, 0:64] = 1.0 / 64
    avg[64:128, 64:128] = 1.0 / 64
    c32 = np.ascontiguousarray(np.stack([ident, sw, avg], axis=1))
    ii, jj = np.meshgrid(np.arange(128), np.arange(128), indexing="ij")
    tri = np.where(ii <= jj, 0.0, -BIG).astype(np.float32)
    cb = np.ascontiguousarray(np.stack([ident, tri], axis=1)).astype(bf)
    s = np.arange(S)
    kconst = np.zeros((2, 64, S), np.float32)
    qconst = np.zeros((4, 64, S), np.float32)
    for ty in range(2):
        b = 0 if ty == 0 else 32
        for j in range(8):
            kconst[ty, b + j] = (s // 256 == j)
        kconst[ty, b + 8] = 1.0
        kconst[ty, b + 9] = 1.0
        kconst[ty, b + 10] = 256.0 * (s // 256)
        kconst[ty, b + 11] = s % 256
    for h in range(4):
        b = 0 if h % 2 == 0 else 32
        sl = SLOPES[h]
        qconst[h, b + 8] = -sl * 256.0 * (s // 256)
        qconst[h, b + 9] = -sl * (s % 256)
        qconst[h, b + 10] = sl
        qconst[h, b + 11] = sl
    return c32, cb, kconst.astype(bf), qconst.astype(bf)


def _prep_inputs(inp):
    f = lambda k: np.asarray(inp[k], dtype=np.float32)
    par = np.zeros((128, NPAR), np.float32)
    gm = np.zeros((4, 2, 8), np.float32)
    for o in range(4):
        gm[o, 0, o + 4:] = -3.0e38
        gm[o, 1, o + 4:] = 3.0e38
    par[:, NL_ALL * NPL:] = gm.reshape(1, 64)
    v8 = lambda v: v.reshape(8, 128).T
    v2 = lambda v: v.reshape(2, 128).T
    cw = lambda w: w.reshape(w.shape[0], 2, 128).transpose(2, 0, 1).reshape(128, w.shape[0] * 2)
    for l in range(NL_ALL):
        b = l * NPL
        par[:, b + 0:b + 8] = v8(f("pre_mix_g")[l])
        par[:, b + 8:b + 16] = v8(f("post_mix_g")[l])
        par[:, b + 16:b + 24] = v8(f("pre_mlp_g")[l])
        par[:, b + 24:b + 32] = v8(f("post_mlp_g")[l])
        par[:, b + 32:b + 34] = v2(f("conf_dw_b")[l])
        par[:, b + 34:b + 36] = v2(f("conf_gn_g")[l])
        par[:, b + 36:b + 38] = v2(f("conf_gn_b")[l])
        par[:, b + 38:b + 40] = v2(f("lru_conv_b")[l])
        par[:, b + 40:b + 42] = v2(f("lru_ba")[l])
        par[:, b + 42:b + 44] = v2(f("lru_bx")[l])
        par[:, b + 44:b + 46] = v2(f("lru_lam")[l])
        par[:, b + 46:b + 108] = cw(f("conf_dw_w")[l])
        par[:, b + 108:b + 114] = cw(f("sconv_w")[l])
        par[:, b + 114:b + 122] = cw(f("lru_conv_w")[l])
    bd = np.zeros((NL_ALL, 128, 4, 128), np.float32)
    for l in range(NL_ALL):
        for wi, nm in enumerate(("lru_wa", "lru_wx")):
            w = f(nm)[l]
            for c in range(2):
                for gi in range(2):
                    bd[l, 64 * gi:64 * gi + 64, wi * 2 + c, 64 * gi:64 * gi + 64] = w[2 * c + gi]
    c32, cb, kconst, qconst = _prep_consts()
    shared = dict(w_in=np.ascontiguousarray(f("w_in")), w_out=np.ascontiguousarray(f("w_out")),
                  w1=np.ascontiguousarray(f("mlp_w1")), w2=np.ascontiguousarray(f("mlp_w2")),
                  par=par, bd=bd, c32=c32, cb=cb, kconst=kconst, qconst=qconst)
    x = f("x")
    return [dict(shared, x=np.ascontiguousarray(x[b])) for b in range(8)]


_NC_CACHE = {}


def kernel(**inputs):
    in_maps = _prep_inputs(inputs)
    if "nc" not in _NC_CACHE:
        _NC_CACHE["nc"] = build_nc(NL_ALL)
    res = run_bass_kernel_spmd(_NC_CACHE["nc"], in_maps, core_ids=list(range(8)))
    return np.stack([np.asarray(r["out"], dtype=np.float32) for r in res.results], axis=0)
```
